# Optimizing a Trainium2 kernel written in Bass

```python
import jax, jax.numpy as jnp
from jax import lax
import numpy as np

D_MODEL = 1024
BATCH = 8
SEQ = 2048
DEPTH = 1

CHUNK = 64
CONV_DIM = D_MODEL
CONV_WIDTH = 31
GLA_HEADS = 4
GLA_DK = D_MODEL // 2
GLA_DV = D_MODEL
GLA_HK = GLA_DK // GLA_HEADS
GLA_HV = GLA_DV // GLA_HEADS
GATE_RANK = 16
GATE_TAU = 16.0
N_GROUPS = 8
EXPERTS_PER_GROUP = 8
N_EXPERTS = N_GROUPS * EXPERTS_PER_GROUP
TOP_K = 2
D_EXPERT = D_MODEL // 2
MOE_BLOCK = 128
LN_EPS = 1e-5
RMS_EPS = 1e-6
DEEPNORM_ALPHA = (2.0 * DEPTH) ** 0.25
DEEPNORM_BETA = (8.0 * DEPTH) ** -0.25
IN_SPLITS = (CONV_DIM, CONV_DIM, GLA_DK, GLA_DK, GLA_DV, GLA_DV, GATE_RANK, D_MODEL, D_MODEL)
D_IN_PROJ = sum(IN_SPLITS)

kernel_name = "hybrid_conv_gla_hmoe_deepnorm"


def layer_norm(x, g, b):
    xf = x.astype(jnp.float32)
    mu = jnp.mean(xf, axis=-1, keepdims=True)
    var = jnp.mean(jnp.square(xf - mu), axis=-1, keepdims=True)
    return ((xf - mu) * lax.rsqrt(var + LN_EPS) * g + b).astype(x.dtype)


def conv_module(a, g, conv_w, conv_b, ln_g, ln_b, w_co, b_co):
    u = a * jax.nn.sigmoid(g)
    u = lax.conv_general_dilated(
        u, conv_w[:, None, :].astype(u.dtype), window_strides=(1,),
        padding=[(CONV_WIDTH - 1, 0)],
        dimension_numbers=("NWC", "WIO", "NWC"),
        feature_group_count=CONV_DIM) + conv_b
    u = jax.nn.silu(layer_norm(u, ln_g, ln_b))
    return u @ w_co + b_co


def gla_branch(q, k, v, r, f_lr, w_gate_up, b_gate_up, norm_g, w_o):
    bsz, seq, _ = q.shape
    n_chunks = seq // CHUNK
    dt = q.dtype
    f32 = jnp.float32
    log_a = jax.nn.log_sigmoid((f_lr @ w_gate_up + b_gate_up).astype(f32)) / GATE_TAU

    def chunked(t, hd):
        return t.astype(f32).reshape(bsz, n_chunks, CHUNK, GLA_HEADS, hd)

    qc = chunked(q, GLA_HK) * (GLA_HK ** -0.5)
    kc = chunked(k, GLA_HK)
    vc = chunked(v, GLA_HV)
    cum = jnp.cumsum(chunked(log_a, GLA_HK), axis=2)
    cum_end = cum[:, :, -1:]
    k_dec = kc * jnp.exp(cum_end - cum)
    chunk_kv = jnp.einsum("bnchk,bnchv->nbhkv", k_dec, vc)
    chunk_decay = jnp.exp(cum_end[:, :, 0]).transpose(1, 0, 2, 3)

    def step(state, inp):
        dec, kv = inp
        state = dec[..., None] * state + kv
        return state, state

    s0 = jnp.zeros((bsz, GLA_HEADS, GLA_HK, GLA_HV), f32)
    _, states = lax.scan(step, s0, (chunk_decay, chunk_kv))
    o = jnp.einsum("bnchk,nbhkv->bnchv", qc, states).reshape(bsz, seq, GLA_HEADS, GLA_HV)
    o = o * lax.rsqrt(jnp.mean(jnp.square(o), axis=-1, keepdims=True) + RMS_EPS) * norm_g
    o = o * jax.nn.silu(r.astype(f32)).reshape(bsz, seq, GLA_HEADS, GLA_HV)
    return o.reshape(bsz, seq, GLA_DV).astype(dt) @ w_o


def hier_moe(x, w_rg, b_rg, w_re, b_re, w1, w3, w2):
    bsz, seq, d = x.shape
    n_tok = bsz * seq
    xt = x.reshape(n_tok, d)
    f32 = jnp.float32
    g_logits = (xt @ w_rg + b_rg).astype(f32)
    g_prob = jax.nn.softmax(g_logits, axis=-1)
    _, g_sel = lax.top_k(g_logits, 1)
    g_w = jnp.take_along_axis(g_prob, g_sel, axis=1)
    e_all = (xt @ w_re + b_re).astype(f32).reshape(n_tok, N_GROUPS, EXPERTS_PER_GROUP)
    e_logits = jnp.take_along_axis(e_all, g_sel[:, :, None], axis=1)[:, 0]
    top_v, top_i = lax.top_k(e_logits, TOP_K)
    weights = g_w * jax.nn.softmax(top_v, axis=-1)
    expert = g_sel * EXPERTS_PER_GROUP + top_i

    n_assign = n_tok * TOP_K
    flat_e = expert.reshape(-1)
    flat_tok = jnp.repeat(jnp.arange(n_tok, dtype=jnp.int32), TOP_K)
    flat_w = weights.reshape(-1)
    order = jnp.argsort(flat_e)
    se, st, sw = flat_e[order], flat_tok[order], flat_w[order]
    counts = jnp.bincount(flat_e, length=N_EXPERTS)
    starts = jnp.cumsum(counts) - counts
    pcounts = (counts + MOE_BLOCK - 1) // MOE_BLOCK * MOE_BLOCK
    pends = jnp.cumsum(pcounts)
    pstarts = pends - pcounts
    dest = pstarts[se] + (jnp.arange(n_assign) - starts[se])
    n_blocks = -(-n_assign // MOE_BLOCK) + N_EXPERTS
    n_slots = n_blocks * MOE_BLOCK
    slot_tok = jnp.zeros((n_slots,), jnp.int32).at[dest].set(st)
    slot_w = jnp.zeros((n_slots,), x.dtype).at[dest].set(sw.astype(x.dtype))
    block_e = jnp.minimum(
        jnp.searchsorted(pends, jnp.arange(n_blocks) * MOE_BLOCK, side="right"), N_EXPERTS - 1)
    xs = xt[slot_tok].reshape(n_blocks, MOE_BLOCK, d)

    def expert_block(args):
        xb, e = args
        hdn = jax.nn.silu(xb @ w1[e]) * (xb @ w3[e])
        return hdn @ w2[e]

    ys = lax.map(expert_block, (xs, block_e)).reshape(n_slots, d) * slot_w[:, None]
    out = jnp.zeros((n_tok, d), x.dtype).at[slot_tok].add(ys)
    return out.reshape(bsz, seq, d)


def setup_inputs(seed: int = 0) -> dict:
    key = jax.random.key(seed)
    ks = jax.random.split(key, 26)
    L = DEPTH

    def nrm(k, shape, scale):
        return jax.random.normal(k, shape, jnp.float32) * scale

    return {
        "x": nrm(ks[0], (BATCH, SEQ, D_MODEL), 1.0),
        "w_in": nrm(ks[1], (L, D_MODEL, D_IN_PROJ), D_MODEL ** -0.5),
        "b_in": nrm(ks[2], (L, D_IN_PROJ), 0.02),
        "conv_w": nrm(ks[3], (L, CONV_WIDTH, CONV_DIM), CONV_WIDTH ** -0.5),
        "conv_b": nrm(ks[4], (L, CONV_DIM), 0.02),
        "conv_ln_g": 1.0 + nrm(ks[5], (L, CONV_DIM), 0.05),
        "conv_ln_b": nrm(ks[6], (L, CONV_DIM), 0.02),
        "w_conv_out": nrm(ks[7], (L, CONV_DIM, D_MODEL), CONV_DIM ** -0.5),
        "b_conv_out": nrm(ks[8], (L, D_MODEL), 0.02),
        "w_gate_up": nrm(ks[9], (L, GATE_RANK, GLA_DK), GATE_RANK ** -0.5),
        "b_gate_up": nrm(ks[10], (L, GLA_DK), 0.1),
        "gla_norm_g": 1.0 + nrm(ks[11], (L, GLA_HEADS, GLA_HV), 0.05),
        "w_gla_out": nrm(ks[12], (L, GLA_DV, D_MODEL), GLA_DV ** -0.5),
        "w_out": nrm(ks[13], (L, D_MODEL, D_MODEL), D_MODEL ** -0.5 * DEEPNORM_BETA),
        "b_out": nrm(ks[14], (L, D_MODEL), 0.02),
        "ln1_g": 1.0 + nrm(ks[15], (L, D_MODEL), 0.05),
        "ln1_b": nrm(ks[16], (L, D_MODEL), 0.02),
        "w_router_group": nrm(ks[17], (L, D_MODEL, N_GROUPS), D_MODEL ** -0.5),
        "b_router_group": nrm(ks[18], (L, N_GROUPS), 0.01),
        "w_router_expert": nrm(ks[19], (L, D_MODEL, N_EXPERTS), D_MODEL ** -0.5),
        "b_router_expert": nrm(ks[20], (L, N_EXPERTS), 0.01),
        "w1": nrm(ks[21], (L, N_EXPERTS, D_MODEL, D_EXPERT), D_MODEL ** -0.5),
        "w3": nrm(ks[22], (L, N_EXPERTS, D_MODEL, D_EXPERT), D_MODEL ** -0.5),
        "w2": nrm(ks[23], (L, N_EXPERTS, D_EXPERT, D_MODEL), D_EXPERT ** -0.5 * DEEPNORM_BETA),
        "ln2_g": 1.0 + nrm(ks[24], (L, D_MODEL), 0.05),
        "ln2_b": nrm(ks[25], (L, D_MODEL), 0.02),
    }


def reference(x, w_in, b_in, conv_w, conv_b, conv_ln_g, conv_ln_b, w_conv_out, b_conv_out,
              w_gate_up, b_gate_up, gla_norm_g, w_gla_out, w_out, b_out, ln1_g, ln1_b,
              w_router_group, b_router_group, w_router_expert, b_router_expert,
              w1, w3, w2, ln2_g, ln2_b):
    split_points = np.cumsum(IN_SPLITS)[:-1].tolist()
    for l in range(DEPTH):
        h = x @ w_in[l] + b_in[l]
        conv_a, conv_g, q, k, v, r, f_lr, gate_a, gate_b = jnp.split(h, split_points, axis=-1)
        y_conv = conv_module(conv_a, conv_g, conv_w[l], conv_b[l], conv_ln_g[l], conv_ln_b[l],
                             w_conv_out[l], b_conv_out[l])
        y_gla = gla_branch(q, k, v, r, f_lr, w_gate_up[l], b_gate_up[l], gla_norm_g[l], w_gla_out[l])
        merged = jax.nn.sigmoid(gate_a) * y_conv + jax.nn.sigmoid(gate_b) * y_gla
        mix = merged @ w_out[l] + b_out[l]
        x = layer_norm(DEEPNORM_ALPHA * x + mix, ln1_g[l], ln1_b[l])
        ffn = hier_moe(x, w_router_group[l], b_router_group[l], w_router_expert[l],
                       b_router_expert[l], w1[l], w3[l], w2[l])
        x = layer_norm(DEEPNORM_ALPHA * x + ffn, ln2_g[l], ln2_b[l])
    return x
```

```python
import contextlib
import numpy as np
import concourse.bass as bass
import concourse.mybir as mybir
from concourse.bass_utils import run_bass_kernel_spmd

F32 = mybir.dt.float32
F32R = mybir.dt.float32r
I32 = mybir.dt.int32
AF = mybir.ActivationFunctionType
ALU = mybir.AluOpType
AX = mybir.AxisListType

NCORES = 8
T = 2048
D = 1024
TT = 256
NTILE = T // TT
NSUB = TT // 128
NCHK = TT // 64
DIN = 7184
NE = 64
CAP = 128
ALPHA = float((2.0 * 1) ** 0.25)
LN_EPS = 1e-5
RMS_EPS = 1e-6
SAME_ENGINE_SYNC = True
DEBUG = False
DBG_STAGE = None
DBG_COLS = 16384


class _Stop(Exception):
    pass

C_A, C_G, C_Q, C_K, C_V, C_R, C_F, C_GA, C_GB = 0, 1024, 2048, 2560, 3072, 4096, 5120, 5136, 6160

V_BA, V_BG, V_BQ, V_BR, V_BGA, V_BGB = 0, 8, 16, 20, 28, 36
V_BF = 44
V_CB, V_CLG, V_CLB, V_BCO, V_NG, V_BO, V_L1G, V_L1B = 45, 53, 61, 69, 77, 85, 93, 101
V_CW = 109
NVEC = V_CW + 8 * 31
K_ID, K_TRIU, K_TRIS, K_IND, K_IOTA = 0, 128, 256, 384, 386
NCONST = 386 + 64


class _Rec:
    def __getattr__(self, name):
        return lambda *a, **k: (name, a, k)


_REC = _Rec()


class Sched:
    def __init__(self, nc, es):
        self.nc = nc
        self.es = es
        self.order = ["pe", "act", "dve", "pool", "sp"]
        self.prog = {k: [] for k in self.order}
        self.esem = {k: es.enter_context(nc.semaphore("sem_" + k)) for k in ("pe", "act", "dve", "pool")}
        self.ecnt = {k: 0 for k in self.esem}
        self.dsem = {}
        self.seen = {k: {} for k in self.order}
        self.lastw = {}
        self.readers = {}
        self.nwaits = 0

    def _collect(self, eng, reads, writes):
        evs = []
        for k in reads:
            w = self.lastw.get(k)
            if w is not None:
                evs.append(w)
        for k in writes:
            w = self.lastw.get(k)
            if w is not None:
                evs.append(w)
            evs.extend(self.readers.get(k, ()))
        best = {}
        seen = self.seen[eng]
        for (sname, sem, val, e_eng) in evs:
            if e_eng == eng and (eng == "pe" or not SAME_ENGINE_SYNC):
                continue
            if seen.get(sname, 0) >= val:
                continue
            if sname not in best or best[sname][1] < val:
                best[sname] = (sem, val)
        waits = []
        for sname, (sem, val) in best.items():
            seen[sname] = val
            waits.append((sem, val))
        self.nwaits += len(waits)
        return waits

    def _commit(self, ev, reads, writes):
        for k in reads:
            self.readers.setdefault(k, []).append(ev)
        for k in writes:
            self.lastw[k] = ev
            self.readers[k] = []

    PSUM_KEYS = ("pm", "pk", "po", "pstat")

    def op(self, eng, fn, reads=(), writes=()):
        writes = list(writes) + [k for k in reads if k[0] in self.PSUM_KEYS]
        reads = [k for k in reads if k[0] not in self.PSUM_KEYS]
        waits = self._collect(eng, reads, writes)
        self.ecnt[eng] += 1
        val = self.ecnt[eng]
        sem = self.esem[eng]
        rec = fn(_REC)

        def emit(e, waits=waits, rec=rec, sem=sem):
            for (s, v) in waits:
                e.wait_ge(s, v)
            getattr(e, rec[0])(*rec[1], **rec[2]).then_inc(sem, 1)

        self.prog[eng].append(emit)
        ev = ("e_" + eng, sem, val, eng)
        self._commit(ev, reads, writes)
        return ev

    def dma(self, q, fn, reads, writes, semkey):
        reads = list(reads)
        writes = list(writes)
        if semkey not in self.dsem:
            self.dsem[semkey] = [self.es.enter_context(self.nc.semaphore("d_" + semkey)), 0]
        ent = self.dsem[semkey]
        sname = "d_" + semkey
        waits = self._collect(q, reads, writes)
        if ent[1] > 0 and self.seen[q].get(sname, 0) < 16 * ent[1]:
            waits = [w for w in waits if w[0] is not ent[0]]
            waits.append((ent[0], 16 * ent[1]))
            self.seen[q][sname] = 16 * ent[1]
        ent[1] += 1
        val = 16 * ent[1]
        sem = ent[0]
        rec = fn(_REC)

        def emit(e, waits=waits, rec=rec, sem=sem):
            for (s, v) in waits:
                e.wait_ge(s, v)
            getattr(e, rec[0])(*rec[1], **rec[2]).then_inc(sem, 16)

        self.prog[q].append(emit)
        ev = (sname, sem, val, None)
        self._commit(ev, reads, writes)
        return ev

    def wait_event(self, eng, ev):
        (sname, sem, val, _) = ev
        if self.seen[eng].get(sname, 0) >= val:
            return
        self.seen[eng][sname] = val

        def emit(e, sem=sem, val=val):
            e.wait_ge(sem, val)

        self.prog[eng].append(emit)

    def emit_all(self, block):
        progs = self.prog

        @block.tensor
        def _(e):
            for f in progs["pe"]:
                f(e)

        @block.scalar
        def _(e):
            for f in progs["act"]:
                f(e)

        @block.vector
        def _(e):
            for f in progs["dve"]:
                f(e)

        @block.gpsimd
        def _(e):
            for f in progs["pool"]:
                f(e)

        @block.sync
        def _(e):
            for f in progs["sp"]:
                f(e)


def build_nc():
    nc = bass.Bass("TRN2", target_bir_lowering=False)

    def din(name, shape, dt=F32):
        return nc.dram_tensor(name, list(shape), dt, kind="ExternalInput").ap()

    x = din("x", [T, D])
    w_in = din("w_in", [D, DIN])
    w_co = din("w_co", [D, D])
    w_gla = din("w_gla", [D, D])
    w_out = din("w_out", [D, D])
    w_gu = din("w_gu", [16, 512])
    w_r = din("w_r", [D, 72])
    w1 = din("w1", [NE, D, 512])
    w3 = din("w3", [NE, D, 512])
    w2 = din("w2", [NE, 512, D])
    cvec_d = din("cvec", [128, NVEC])
    const_d = din("consts", [128, NCONST])
    brow_d = din("brow", [4, 512])
    brep_d = din("brep", [128, 72])
    g2b2_d = din("g2b2", [128, 2 * D])
    out = nc.dram_tensor("out", [T, D], F32, kind="ExternalOutput").ap()
    dbg_kind = "ExternalOutput" if DEBUG else "Internal"
    x1d = nc.dram_tensor("x1d", [T, D], F32, kind=dbg_kind).ap()
    rtd = nc.dram_tensor("rtd", [128, 16 * 4], F32, kind=dbg_kind).ap()
    nrows_scr = 128 if DBG_STAGE else NE * CAP
    xs_d = nc.dram_tensor("xs_d", [nrows_scr, D], F32, kind="Internal").ap()
    dbg_d = nc.dram_tensor("dbg", [128, DBG_COLS], F32, kind="ExternalOutput").ap() if DBG_STAGE else None
    dbg_layout = {}
    nc.dbg_layout = dbg_layout
    ys_d = nc.dram_tensor("ys_d", [nrows_scr, D], F32, kind="Internal").ap()

    with contextlib.ExitStack() as es:
        S = Sched(nc, es)

        def sb(name, shape, dt=F32):
            return es.enter_context(nc.sbuf_tensor(name, list(shape), dt))

        def ps(name, shape):
            return es.enter_context(nc.psum_tensor(name, list(shape), F32))

        NSLOT = 8
        SL = 2048
        POOL = sb("pool", [128, NSLOT * SL])
        RPOOL = sb("rpool", [128, 7 * SL], F32R)
        WBT = [sb(f"wb{r}", [128, 8 * 512], F32R) for r in range(3)]
        UT = sb("ut", [128, 8, TT + 30])
        CVEC = sb("cvec_t", [128, NVEC])
        CONST = sb("const_t", [128, NCONST])
        ONES = sb("ones", [128, 128])
        ONESR = sb("onesr", [128, 128], F32R)
        BREP = sb("brep_t", [128, 72])
        WRT = sb("wr_t", [128, 8, 72])
        WGU = sb("wgu_t", [32, 512])
        WFL = sb("wfl_t", [128, 8, 16])
        EPS = sb("eps_t", [128, 2])
        MU = sb("mu", [128, TT])
        MSQ = sb("msq", [128, TT])
        SD = MSQ
        RSTD = MSQ
        DEC = sb("dec", [128, NSUB * 4 * 2])
        FLT = sb("flt", [32, TT])
        L2S = sb("l2s", [128, 16, 8])
        ST = [sb(f"st{i}", [128, 4, 256]) for i in range(2)]
        LG = sb("lg", [128, NSUB, 72])
        RUN = sb("run", [128, 64])
        GIDX = sb("gidx", [128, 16, 2], I32)
        SIDX = sb("sidx", [128, 16, 2], I32)
        WAB = sb("wab", [128, 16, 2])
        RTF = sb("rtf", [128, 16, 4]) if DEBUG else None
        R_ = {n: sb("r_" + n, [128, w]) for n, w in [
            ("gmax", 1), ("ngmax", 1), ("ohg", 8), ("ge", 8), ("gsum", 1), ("gw", 1), ("t64", 64), ("esel", 8),
            ("m1", 1), ("oh1", 8), ("es2", 8), ("m2", 1), ("oh2", 8), ("d", 1), ("e2", 1), ("den", 1), ("p1", 1),
            ("p2", 1), ("oha", 64), ("ohb", 64), ("ohs", 64), ("cnt", 64), ("pos", 2), ("eid", 2), ("val", 2),
            ("slot", 2), ("sidf", 2), ("gidf", 2), ("tmp2", 2)]}

        PM = [ps(f"pm{i}", [128, 512]) for i in range(4)]
        PK = ps("pk", [128, 1024])
        PO = ps("po", [128, 512])
        PS_ = ps("pstat", [128, 512])

        def slot(i, lo=0, hi=SL):
            return POOL[:, i * SL + lo:i * SL + hi]

        def fmv(i, dt=None):
            ap = slot(i).rearrange("p (c t) -> p c t", c=8)
            return ap.bitcast(dt) if dt is not None else ap

        def pk(i, lo=0, hi=SL):
            return [("P", i, g) for g in range(lo // 256, (hi + 255) // 256)]

        def wbv(r):
            return WBT[r][:, :].rearrange("p (k n) -> p k n", k=8)

        def rslot(i, lo=0, hi=SL):
            return RPOOL[:, i * SL + lo:i * SL + hi]

        def rmv(i):
            return rslot(i).rearrange("p (c t) -> p c t", c=8)

        def rk(i, lo=0, hi=SL):
            return [("R", i, g) for g in range(lo // 256, (hi + 255) // 256)]

        BROWS = [rslot(6, j * 512, (j + 1) * 512) for j in range(3)]
        SG = slot(3, 0, 512)

        def wbk(r):
            return [("WB", r)]

        def cv(col, n=1):
            return CVEC[:, col:col + n]

        pm_ctr = [0]

        def next_pm():
            i = pm_ctr[0] % 4
            pm_ctr[0] += 1
            return i

        S.dma("sp", lambda e: e.dma_start(out=CVEC[:, :], in_=cvec_d), [], [("cvec",)], "c0")
        S.dma("sp", lambda e: e.dma_start(out=CONST[:, :], in_=const_d), [], [("const",)], "c1")
        S.dma("sp", lambda e: e.dma_start(out=BREP[:, :], in_=brep_d), [], [("brep",)], "c2")
        S.dma("sp", lambda e: e.dma_start(out=WRT[:, :, :], in_=w_r.rearrange("(k p) n -> p k n", p=128)),
              [], [("wrt",)], "c3")
        S.op("dve", lambda e: e.memset(WGU[:, :], 0.0), [], [("wgu",)])
        S.dma("sp", lambda e: e.dma_start(out=WGU[0:16, :], in_=w_gu), [], [("wgu",)], "c0")
        S.dma("sp", lambda e: e.dma_start(out=WFL[:, :, :],
                                          in_=w_in[:, C_F:C_F + 16].rearrange("(k p) n -> p k n", p=128)),
              [], [("wfl",)], "c1")
        S.dma("sp", lambda e: e.dma_start(out=WGU[16:17, :], in_=brow_d[3:4, :]), [("wgu",)], [("wgu",)], "c2")
        S.op("dve", lambda e: e.memset(FLT[:, :], 1.0), [], [("flt",)])
        for j in range(3):
            S.dma("pool", lambda e, j=j: e.dma_start(out=BROWS[j][0:1, :], in_=brow_d[j:j + 1, :]),
                  [], rk(6, j * 512, (j + 1) * 512), "c4")
        S.op("dve", lambda e: e.memset(ONES[:, :], 1.0), [], [("ones",)])
        S.op("dve", lambda e: e.tensor_copy(out=ONESR[:, :], in_=ONES[:, :]), [("ones",)], [("onesr",)])
        S.op("dve", lambda e: e.memset(EPS[:, 0:1], LN_EPS), [], [("eps",)])
        S.op("dve", lambda e: e.memset(EPS[:, 1:2], RMS_EPS), [("eps",)], [("eps",)])
        S.op("dve", lambda e: e.memset(ST[0][:, :, :], 0.0), [], [("st", 0, h) for h in range(4)])
        S.op("dve", lambda e: e.memset(RUN[:, :], 0.0), [], [("run",)])
        S.op("dve", lambda e: e.memset(UT[:, :, 0:30], 0.0), [], [("uhalo",)])
        if not DBG_STAGE:
            S.op("dve", lambda e: e.memset(slot(7), 0.0), [], pk(7))
            S.op("dve", lambda e: e.tensor_copy(out=rslot(4), in_=slot(7)), pk(7), rk(4))
            ZSRC = rslot(4).bitcast(F32)
        zero_next = [0]

        def issue_zero(n_):
            for _ in range(n_):
                g_ = zero_next[0]
                if g_ >= 32 or DBG_STAGE:
                    return
                S.dma("sp", lambda e, g_=g_: e.dma_start(
                    out=xs_d[g_ * 256:(g_ + 1) * 256, :].rearrange("(e p) d -> p e d", p=128),
                    in_=ZSRC.rearrange("p (e d) -> p e d", e=2)), rk(4), [("xs",)], f"z{g_ % 4}")
                zero_next[0] += 1
        IDENT = CONST[:, K_ID:K_ID + 128]
        TRIU = CONST[:, K_TRIU:K_TRIU + 128]
        TRIS = CONST[:, K_TRIS:K_TRIS + 128]
        IND = CONST[:, K_IND:K_IND + 2]
        IOTA = CONST[:, K_IOTA:K_IOTA + 64]

        def wsrc(name, c0):
            m = {"in": w_in, "co": w_co, "gla": w_gla, "out": w_out}[name]
            return m[:, c0:c0 + 512].rearrange("(k p) n -> p k n", p=128)

        blk_list = []
        for it in range(NTILE):
            for nm, c0 in [("in", C_G), ("in", C_G + 512), ("in", C_A), ("in", C_A + 512),
                           ("in", C_K), ("in", C_V), ("in", C_V + 512), ("in", C_Q),
                           ("in", C_GA), ("in", C_GA + 512), ("in", C_R), ("in", C_R + 512),
                           ("in", C_GB), ("in", C_GB + 512), ("gla", 0), ("gla", 512),
                           ("co", 0), ("co", 512),
                           ("out", 0), ("out", 512)]:
                blk_list.append((nm, c0))
        NBLK = len(blk_list)
        blk_issued = [0]
        blk_used = [0]

        def issue_block():
            j = blk_issued[0]
            if j >= NBLK:
                return
            nm, c0 = blk_list[j]
            r = j % 3
            S.dma("pool", lambda e, r=r, nm=nm, c0=c0: e.dma_start(out=wbv(r), in_=wsrc(nm, c0)),
                  [], wbk(r), f"wb{r}")
            blk_issued[0] += 1

        def take_block(nm, c0):
            j = blk_used[0]
            assert blk_list[j] == (nm, c0), (j, blk_list[j], nm, c0)
            blk_used[0] += 1
            return j % 3

        def release_block():
            issue_block()
            issue_zero(4)

        issue_block()
        issue_block()
        issue_block()

        def fm_block(nm, c0, rhs_slot, evac):
            r = take_block(nm, c0)
            W = wbv(r)
            X = rmv(rhs_slot)
            for fc in range(4):
                b = next_pm()
                for k in range(8):
                    S.op("pe", lambda e, b=b, k=k, fc=fc, W=W, X=X: e.matmul(
                        PM[b][:, 0:TT], W[:, k, fc * 128:(fc + 1) * 128], X[:, k, :],
                        start=(k == 0), stop=(k == 7)),
                        wbk(r) + rk(rhs_slot), [("pm", b)])
                evac(b, (c0 % 1024) // 128 + fc)
            release_block()

        def tm_block(nm, c0, bias_j, evac):
            r = take_block(nm, c0)
            W = wbv(r)
            X = rmv(R_XTR)
            for s in range(NSUB):
                b = next_pm()
                for k in range(8):
                    S.op("pe", lambda e, b=b, k=k, s=s, W=W, X=X: e.matmul(
                        PM[b][:, :], X[:, k, s * 128:(s + 1) * 128], W[:, k, :],
                        start=(k == 0), stop=False),
                        wbk(r) + rk(R_XTR), [("pm", b)])
                S.op("pe", lambda e, b=b, j=bias_j: e.matmul(
                    PM[b][:, :], ONESR[0:1, :], BROWS[j][0:1, :],
                    start=False, stop=True), [("onesr",)] + rk(6, bias_j * 512, (bias_j + 1) * 512), [("pm", b)])
                evac(b, s)
            release_block()

        def ln_stats(src, eps_col):
            SRC = fmv(src)
            SQ = rmv(R_SQ)
            S.op("act", lambda e: e.activation(out=rslot(R_SQ), in_=slot(src), func=AF.Square),
                 pk(src), rk(R_SQ))
            for k in range(8):
                S.op("pe", lambda e, k=k: e.matmul(PS_[:, 0:TT], ONES[:, :], SRC[:, k, :],
                                                   start=(k == 0), stop=(k == 7)),
                     [("ones",)] + pk(src), [("pstat",)])
            S.op("act", lambda e: e.activation(out=MU[:, :], in_=PS_[:, 0:TT], func=AF.Copy, scale=1.0 / D),
                 [("pstat",)], [("mu",)])
            for k in range(8):
                S.op("pe", lambda e, k=k: e.matmul(PS_[:, TT:2 * TT], ONESR[:, :], SQ[:, k, :],
                                                   start=(k == 0), stop=(k == 7)),
                     [("onesr",)] + rk(R_SQ), [("pstat",)])
            S.op("dve", lambda e: e.tensor_tensor(out=MSQ[:, :], in0=MU[:, :], in1=MU[:, :], op=ALU.mult),
                 [("mu",)], [("msq",)])
            S.op("dve", lambda e: e.scalar_tensor_tensor(out=MSQ[:, :], in0=PS_[:, TT:2 * TT], scalar=1.0 / D,
                                                         in1=MSQ[:, :], op0=ALU.mult, op1=ALU.subtract),
                 [("pstat",), ("msq",)], [("msq",)])
            S.op("act", lambda e: e.activation(out=SD[:, :], in_=MSQ[:, :], func=AF.Sqrt,
                                               bias=EPS[:, eps_col:eps_col + 1], scale=1.0),
                 [("msq",), ("eps",)], [("msq",)])
            S.op("dve", lambda e: e.reciprocal(out=RSTD[:, :], in_=SD[:, :]), [("msq",)], [("msq",)])
            MUB = MU[:, :].unsqueeze(1).to_broadcast([128, 8, TT])
            RSB = RSTD[:, :].unsqueeze(1).to_broadcast([128, 8, TT])
            S.op("dve", lambda e: e.tensor_tensor(out=SRC, in0=SRC, in1=MUB, op=ALU.subtract),
                 pk(src) + [("mu",)], pk(src))
            S.op("dve", lambda e: e.tensor_tensor(out=SRC, in0=SRC, in1=RSB, op=ALU.mult),
                 pk(src) + [("msq",)], pk(src))

        P_CVX = P_ZX = 0
        P_XT32 = 1
        P_OTX = P_X1TX = 2
        P_G = P_GA = 3
        P_XTOK = P_QT = 4
        P_SRX = 6
        P_GBX = 7
        P_LA = P_M = 5
        P_X1KX = 6
        R_XTR, R_KT, R_V, R_CSR, R_OGR, R_SQ, R_X = 0, 1, 2, 3, 4, 5, 6
        R_MR = R_OGR

        dbg_evs = []

        def stage(name, it, bufs):
            if DBG_STAGE is None or DBG_STAGE != (name, it):
                return
            off = 0
            for (label, ap, keys) in bufs:
                shp = list(ap.shape)
                n = 1
                for d_ in shp[1:]:
                    n *= d_
                npart = shp[0]
                dst = dbg_d[0:npart, off:off + n]
                if len(shp) == 3:
                    dst = dst.rearrange("p (a b) -> p a b", a=shp[1])
                if ap.dtype == F32R:
                    ap = ap.bitcast(F32)
                ev = S.dma("pool", lambda e, dst=dst, ap=ap: e.dma_start(out=dst, in_=ap), keys,
                           [("dbg", label)], "dbg")
                dbg_evs.append(ev)
                dbg_layout[label] = (off, shp)
                off += n
            assert off <= DBG_COLS, off
            raise _Stop()

        def phase1_tile(it):
            t0 = it * TT
            stage("pro", it, [("cvec", CVEC[:, 0:128], [("cvec",)]), ("onesr", ONESR[:, :], [("onesr",)]),
                              ("brow", BROWS[0][0:1, :], rk(6, 0, 512)), ("wfl", WFL[:, :, :], [("wfl",)]),
                              ("ut", UT[:, :, 0:30], [("uhalo",)])])
            XTOK = slot(P_XTOK).rearrange("p (s d) -> p s d", s=NSUB)
            S.dma("sp", lambda e, t0=t0, XTOK=XTOK: e.dma_start(
                out=XTOK, in_=x[t0:t0 + TT, :].rearrange("(s p) d -> p s d", p=128)),
                [], pk(P_XTOK), "xin")
            XT32 = fmv(P_XT32)
            XTR = rmv(R_XTR)
            stage("xl", it, [("xtok", XTOK, pk(P_XTOK))])
            for s in range(NSUB):
                for cg in range(2):
                    for cc in range(4):
                        c = cg * 4 + cc
                        S.op("pe", lambda e, s=s, c=c, cc=cc, XTOK=XTOK: e.transpose(
                            PO[:, cc * 128:(cc + 1) * 128], XTOK[:, s, c * 128:(c + 1) * 128], IDENT),
                            pk(P_XTOK) + [("const",)], [("po",)])
                    pov = PO[:, :].rearrange("p (c t) -> p c t", c=4)
                    import os
                    if os.environ.get("XTR_ENG", "act") == "act":
                        S.op("act", lambda e, s=s, cg=cg, pov=pov, XTR=XTR: e.activation(
                            out=XTR[:, cg * 4:cg * 4 + 4, s * 128:(s + 1) * 128], in_=pov, func=AF.Copy),
                            [("po",)], rk(R_XTR))
                    else:
                        S.op("dve", lambda e, s=s, cg=cg, pov=pov, XTR=XTR: e.tensor_copy(
                            out=XTR[:, cg * 4:cg * 4 + 4, s * 128:(s + 1) * 128], in_=pov),
                            [("po",)], rk(R_XTR))
                    if os.environ.get("XTR_ENG", "act") == "act":
                        S.op("dve", lambda e, s=s, cg=cg, pov=pov, XT32=XT32: e.tensor_copy(
                            out=XT32[:, cg * 4:cg * 4 + 4, s * 128:(s + 1) * 128], in_=pov),
                            [("po",)], pk(P_XT32))
                    else:
                        for cc in range(4):
                            S.op("act", lambda e, s=s, cg=cg, cc=cc, XT32=XT32: e.activation(
                                out=XT32[:, cg * 4 + cc, s * 128:(s + 1) * 128], in_=PO[:, cc * 128:(cc + 1) * 128],
                                func=AF.Copy), [("po",)], pk(P_XT32))

            stage("a", it, [("xtr", XTR, rk(R_XTR)), ("xt32", XT32, pk(P_XT32))])
            yield "early1"
            LA = slot(P_LA, 0, 1024).rearrange("p (s n) -> p s n", s=NSUB)
            DK = slot(P_LA, 1024, 2048).rearrange("p (s n) -> p s n", s=NSUB)
            SG2 = slot(P_QT, 1024, 1536)
            K_SG2 = pk(P_QT, 1024, 1536)

            def step_flr():
                for k in range(8):
                    S.op("pe", lambda e, k=k: e.matmul(PS_[0:16, 0:TT], WFL[:, k, :], XT32[:, k, :],
                                                       start=(k == 0), stop=(k == 7)),
                         [("wfl",)] + pk(P_XT32), [("pstat",)])
                S.op("act", lambda e: e.activation(out=FLT[0:16, :], in_=PS_[0:16, 0:TT], func=AF.Identity,
                                                   bias=CVEC[0:16, V_BF:V_BF + 1], scale=1.0),
                     [("pstat",), ("cvec",)], [("flt",)])

            def step_z(s):
                S.op("pe", lambda e: e.matmul(PS_[:, :], FLT[0:17, s * 128:(s + 1) * 128], WGU[0:17, :],
                                              start=True, stop=True),
                     [("flt",), ("wgu",)], [("pstat",)])
                S.op("act", lambda e: e.activation(out=SG2, in_=PS_[:, :], func=AF.Sigmoid),
                     [("pstat",)], K_SG2)
                S.op("act", lambda e: e.activation(out=LA[:, s, :], in_=SG2, func=AF.Ln),
                     K_SG2, pk(P_LA, s * 512, (s + 1) * 512))

            def step_cum(s):
                S.op("pe", lambda e: e.matmul(PS_[:, :], TRIU, LA[:, s, :], start=True, stop=True),
                     [("const",)] + pk(P_LA, s * 512, (s + 1) * 512), [("pstat",)])
                S.op("act", lambda e: e.activation(out=DK[:, s, :], in_=PS_[:, :], func=AF.Exp,
                                                   scale=1.0 / 16.0),
                     [("pstat",)], pk(P_LA, 1024 + s * 512, 1024 + (s + 1) * 512))

            def step_dec():
                for s in range(NSUB):
                    for h in range(4):
                        o = (s * 4 + h) * 2
                        S.op("pe", lambda e, s=s, h=h, o=o: e.matmul(
                            PS_[:, o:o + 2], LA[:, s, h * 128:(h + 1) * 128], IND, start=True, stop=True),
                            [("const",)] + pk(P_LA, s * 512, (s + 1) * 512), [("pstat",)])
                S.op("act", lambda e: e.activation(out=DEC[:, :], in_=PS_[:, 0:NSUB * 8], func=AF.Exp,
                                                   scale=1.0 / 16.0),
                     [("pstat",)], [("dec",)])

            G = fmv(P_G)

            def ev_g(b, f):
                S.op("act", lambda e: e.activation(out=G[:, f, :], in_=PM[b][:, 0:TT], func=AF.Sigmoid,
                                                   bias=cv(V_BG + f), scale=1.0),
                     [("pm", b), ("cvec",)], pk(P_G, f * 256, (f + 1) * 256))

            def ev_a(b, f):
                S.op("dve", lambda e: e.scalar_tensor_tensor(
                    out=UT[:, f, 30:30 + TT], in0=PM[b][:, 0:TT], scalar=cv(V_BA + f), in1=G[:, f, :],
                    op0=ALU.add, op1=ALU.mult),
                    [("pm", b), ("cvec",), ("uhalo",)] + pk(P_G, f * 256, (f + 1) * 256), [("u", f)])

            KT = rslot(R_KT, 1024, 2048).rearrange("p (s n) -> p s n", s=NSUB)
            QT = slot(P_QT, 0, 1024).rearrange("p (h t) -> p h t", h=4)
            VT = rslot(R_V).rearrange("p (s n) -> p s n", s=NSUB)

            def ev_k(b, s):
                S.op("dve", lambda e: e.tensor_tensor(out=KT[:, s, :], in0=PM[b][:, :], in1=DK[:, s, :],
                                                      op=ALU.mult),
                     [("pm", b)] + pk(P_LA, 1024 + s * 512, 1024 + (s + 1) * 512),
                     rk(R_KT, 1024 + s * 512, 1024 + (s + 1) * 512))

            def ev_q(b, f):
                S.op("act", lambda e: e.activation(out=QT[:, f, :], in_=PM[b][:, 0:TT], func=AF.Identity,
                                                   bias=cv(V_BQ + f), scale=1.0),
                     [("pm", b), ("cvec",)], pk(P_QT, f * 256, (f + 1) * 256))

            fm_block("in", C_G, R_XTR, ev_g)
            step_flr()
            fm_block("in", C_G + 512, R_XTR, ev_g)
            step_z(0)
            if it > 0:
                S.op("dve", lambda e: e.tensor_copy(out=UT[:, :, 0:30], in_=UT[:, :, TT:TT + 30]),
                     [("u", c) for c in range(8)] + [("uhalo",)], [("uhalo",)])
            fm_block("in", C_A, R_XTR, ev_a)
            step_z(1)
            fm_block("in", C_A + 512, R_XTR, ev_a)
            step_cum(0)
            step_cum(1)
            step_dec()
            stage("la", it, [("la", LA, pk(P_LA, 0, 1024)), ("dk", DK, pk(P_LA, 1024, 2048)),
                             ("dec", DEC[:, :], [("dec",)]), ("flt", FLT[:, :], [("flt",)])])
            stage("u", it, [("g", G, pk(P_G)), ("ut", UT[:, :, :], [("u", c) for c in range(8)] + [("uhalo",)])])
            tm_block("in", C_K, 0, ev_k)
            for half in range(2):
                def ev_v(b, s, half=half):
                    S.op("act", lambda e: e.activation(out=VT[:, s, half * 512:(half + 1) * 512],
                                                       in_=PM[b][:, :], func=AF.Copy),
                         [("pm", b)], rk(R_V, s * 1024 + half * 512, s * 1024 + (half + 1) * 512))
                tm_block("in", C_V + 512 * half, 1 + half, ev_v)
            fm_block("in", C_Q, R_XTR, ev_q)

            stage("kvq", it, [("kt", KT, rk(R_KT, 1024, 2048)), ("vt", VT, rk(R_V)), ("qt", QT, pk(P_QT, 0, 1024))])
            yield "early"
            GA = fmv(P_GA)
            for half in range(0):
                def ev_ga(b, f, half=half):
                    ff = half * 4 + f % 4
                    S.op("act", lambda e: e.activation(out=GA[:, ff, :], in_=PM[b][:, 0:TT], func=AF.Sigmoid,
                                                       bias=cv(V_BGA + ff), scale=1.0),
                         [("pm", b), ("cvec",)], pk(P_GA, ff * 256, (ff + 1) * 256))
                fm_block("in", C_GA + 512 * half, R_XTR, ev_ga)

            SR = fmv(P_SRX)
            for half in range(0):
                def ev_r(b, f):
                    S.op("act", lambda e: e.activation(out=SR[:, f, :], in_=PM[b][:, 0:TT], func=AF.Silu,
                                                       bias=cv(V_BR + f), scale=1.0),
                         [("pm", b), ("cvec",)], pk(P_SRX, f * 256, (f + 1) * 256))
                fm_block("in", C_R + 512 * half, R_XTR, ev_r)
            GB = fmv(P_GBX)
            for half in range(0):
                def ev_gb(b, f):
                    S.op("act", lambda e: e.activation(out=GB[:, f, :], in_=PM[b][:, 0:TT], func=AF.Sigmoid,
                                                       bias=cv(V_BGB + f), scale=1.0),
                         [("pm", b), ("cvec",)], pk(P_GBX, f * 256, (f + 1) * 256))
                fm_block("in", C_GB + 512 * half, R_XTR, ev_gb)
            CVT = fmv(P_CVX)

            def conv_part(jlo, jhi):
                for j in range(jlo, jhi):
                    for c in range(8):
                        wcol = cv(V_CW + c * 31 + j)
                        if j == 0:
                            S.op("dve", lambda e, c=c, wcol=wcol: e.tensor_scalar(
                                out=CVT[:, c, :], in0=UT[:, c, 0:TT], scalar1=wcol, scalar2=cv(V_CB + c),
                                op0=ALU.mult, op1=ALU.add),
                                [("u", c), ("uhalo",), ("cvec",)], pk(P_CVX, c * 256, (c + 1) * 256))
                        else:
                            S.op("dve", lambda e, c=c, j=j, wcol=wcol: e.scalar_tensor_tensor(
                                out=CVT[:, c, :], in0=UT[:, c, j:j + TT], scalar=wcol, in1=CVT[:, c, :],
                                op0=ALU.mult, op1=ALU.add),
                                [("u", c), ("uhalo",), ("cvec",)] + pk(P_CVX, c * 256, (c + 1) * 256),
                                pk(P_CVX, c * 256, (c + 1) * 256))

            OT = fmv(P_OTX)

            def blocks_ga():
                for half in range(2):
                    def ev_ga(b, f):
                        S.op("act", lambda e: e.activation(out=GA[:, f, :], in_=PM[b][:, 0:TT], func=AF.Sigmoid,
                                                           bias=cv(V_BGA + f), scale=1.0),
                             [("pm", b), ("cvec",)], pk(P_GA, f * 256, (f + 1) * 256))
                    fm_block("in", C_GA + 512 * half, R_XTR, ev_ga)

            def blocks_r():
                for half in range(2):
                    def ev_r(b, f):
                        S.op("act", lambda e: e.activation(out=SR[:, f, :], in_=PM[b][:, 0:TT], func=AF.Silu,
                                                           bias=cv(V_BR + f), scale=1.0),
                             [("pm", b), ("cvec",)], pk(P_SRX, f * 256, (f + 1) * 256))
                    fm_block("in", C_R + 512 * half, R_XTR, ev_r)

            def blocks_gb():
                for half in range(2):
                    def ev_gb(b, f):
                        S.op("act", lambda e: e.activation(out=GB[:, f, :], in_=PM[b][:, 0:TT], func=AF.Sigmoid,
                                                           bias=cv(V_BGB + f), scale=1.0),
                             [("pm", b), ("cvec",)], pk(P_GBX, f * 256, (f + 1) * 256))
                    fm_block("in", C_GB + 512 * half, R_XTR, ev_gb)

            def gla_kv(n):
                s, hf = n // 2, n % 2
                p0 = hf * 64
                g = it * NCHK + n
                So, Sn = ST[g % 2], ST[(g + 1) % 2]
                for h in range(4):
                    S.op("pe", lambda e, s=s, h=h, p0=p0: e.matmul(
                        PK[:, h * 256:(h + 1) * 256], KT[p0:p0 + 64, s, h * 128:(h + 1) * 128],
                        VT[p0:p0 + 64, s, h * 256:(h + 1) * 256], start=True, stop=True),
                        rk(R_KT, 1024 + s * 512, 1024 + (s + 1) * 512) + rk(R_V, s * 1024, (s + 1) * 1024),
                        [("pk", h // 2)])
                for h in range(4):
                    dcol = (s * 4 + h) * 2 + hf
                    S.op("dve", lambda e, h=h, dcol=dcol, So=So, Sn=Sn: e.scalar_tensor_tensor(
                        out=Sn[:, h, :], in0=So[:, h, :], scalar=DEC[:, dcol:dcol + 1],
                        in1=PK[:, h * 256:(h + 1) * 256], op0=ALU.mult, op1=ALU.add),
                        [("pk", h // 2), ("dec",), ("st", g % 2, h)],
                        [("st", (g + 1) % 2, h)])

            def gla_o(n):
                g = it * NCHK + n
                Sn = ST[(g + 1) % 2]
                for h in range(4):
                    for j in range(2):
                        hj = h * 2 + j
                        S.op("pe", lambda e, h=h, j=j, hj=hj, n=n, Sn=Sn: e.matmul(
                            PO[:, hj * 64:(hj + 1) * 64], Sn[:, h, j * 128:(j + 1) * 128],
                            QT[:, h, n * 64:(n + 1) * 64], start=True, stop=True),
                            [("st", (g + 1) % 2, h)] + pk(P_QT, h * 256, (h + 1) * 256), [("po",)])
                S.op("act", lambda e, n=n: e.activation(
                    out=OT[:, :, n * 64:(n + 1) * 64], in_=PO[:, :].rearrange("p (c t) -> p c t", c=8),
                    func=AF.Copy, scale=float(128.0 ** -0.5)),
                    [("po",)], pk(P_OTX))

            SQ2 = rmv(R_SQ)
            RS = slot(P_QT, 1024, 2048).rearrange("p (h t) -> p h t", h=4)
            K_RS = pk(P_QT, 1024, 2048)
            OGR = rmv(R_OGR)
            MR = rmv(R_MR)

            def h_pre():
                S.op("act", lambda e: e.activation(out=rslot(R_SQ), in_=slot(P_OTX), func=AF.Square),
                     pk(P_OTX), rk(R_SQ))
                for hp in range(2):
                    for hh in range(2):
                        h = hp * 2 + hh
                        for j in range(2):
                            S.op("pe", lambda e, h=h, hh=hh, j=j: e.matmul(
                                PS_[:, hh * TT:(hh + 1) * TT], ONESR[:, :], SQ2[:, h * 2 + j, :],
                                start=(j == 0), stop=(j == 1)),
                                [("onesr",)] + rk(R_SQ), [("pstat",)])
                    S.op("act", lambda e, hp=hp: e.activation(
                        out=RS[:, hp * 2:hp * 2 + 2, :], in_=PS_[:, :].rearrange("p (h t) -> p h t", h=2),
                        func=AF.Sqrt, bias=EPS[:, 1:2], scale=1.0 / 256.0),
                        [("pstat",), ("eps",)], pk(P_QT, 1024 + hp * 512, 1024 + (hp + 1) * 512))

            def h_dve():
                S.op("dve", lambda e: e.reciprocal(out=RS, in_=RS), K_RS, K_RS)
                for h in range(4):
                    S.op("dve", lambda e, h=h: e.tensor_tensor(
                        out=OT[:, 2 * h:2 * h + 2, :], in0=OT[:, 2 * h:2 * h + 2, :],
                        in1=RS[:, h, :].unsqueeze(1).to_broadcast([128, 2, TT]), op=ALU.mult),
                        pk(P_OTX, h * 512, (h + 1) * 512) + K_RS, pk(P_OTX, h * 512, (h + 1) * 512))
                for hj in range(8):
                    S.op("dve", lambda e, hj=hj: e.scalar_tensor_tensor(
                        out=OGR[:, hj, :], in0=OT[:, hj, :], scalar=cv(V_NG + hj), in1=SR[:, hj, :],
                        op0=ALU.mult, op1=ALU.mult),
                        pk(P_OTX, hj * 256, (hj + 1) * 256) + pk(P_SRX, hj * 256, (hj + 1) * 256) + [("cvec",)],
                        rk(R_OGR, hj * 256, (hj + 1) * 256))

            def blocks_gla():
                for half in range(2):
                    def ev_t(b, f):
                        S.op("dve", lambda e: e.tensor_tensor(out=GB[:, f, :], in0=PM[b][:, 0:TT],
                                                              in1=GB[:, f, :], op=ALU.mult),
                             [("pm", b)] + pk(P_GBX, f * 256, (f + 1) * 256), pk(P_GBX, f * 256, (f + 1) * 256))
                    fm_block("gla", 512 * half, R_OGR, ev_t)

            conv_part(0, 4)
            gla_kv(0)
            blocks_ga()
            gla_o(0)
            gla_kv(1)
            conv_part(4, 12)
            blocks_r()
            gla_o(1)
            gla_kv(2)
            conv_part(12, 20)
            blocks_gb()
            gla_o(2)
            gla_kv(3)
            conv_part(20, 24)
            gla_o(3)
            stage("ot", it, [("ot", OT, pk(P_OTX)), ("st0", ST[0][:, :, :], [("st", 0, h) for h in range(4)]),
                             ("st1", ST[1][:, :, :], [("st", 1, h) for h in range(4)])])
            h_pre()
            conv_part(24, 28)
            h_dve()
            stage("ogr", it, [("ogr", OGR, rk(R_OGR)), ("sr", SR, pk(P_SRX))])
            blocks_gla()
            conv_part(28, 31)
            stage("conv", it, [("cv", CVT, pk(P_CVX))])
            ln_stats(P_CVX, 0)
            CSR = rmv(R_CSR)
            for c in range(8):
                S.op("act", lambda e, c=c: e.activation(out=CSR[:, c, :], in_=CVT[:, c, :], func=AF.Silu,
                                                        bias=cv(V_CLB + c), scale=cv(V_CLG + c)),
                     pk(P_CVX, c * 256, (c + 1) * 256) + [("cvec",)], rk(R_CSR, c * 256, (c + 1) * 256))

            stage("csr", it, [("csr", CSR, rk(R_CSR))])
            for half in range(2):
                def ev_m(b, f):
                    S.op("dve", lambda e: e.scalar_tensor_tensor(
                        out=GA[:, f, :], in0=PM[b][:, 0:TT], scalar=cv(V_BCO + f), in1=GA[:, f, :],
                        op0=ALU.add, op1=ALU.mult),
                        [("pm", b), ("cvec",)] + pk(P_GA, f * 256, (f + 1) * 256), pk(P_GA, f * 256, (f + 1) * 256))
                    S.op("dve", lambda e: e.tensor_tensor(out=MR[:, f, :], in0=GA[:, f, :], in1=GB[:, f, :],
                                                          op=ALU.add),
                         pk(P_GA, f * 256, (f + 1) * 256) + pk(P_GBX, f * 256, (f + 1) * 256),
                         rk(R_MR, f * 256, (f + 1) * 256))
                fm_block("co", 512 * half, R_CSR, ev_m)

            stage("mr", it, [("mr", MR, rk(R_MR))])
            Z = fmv(P_ZX)
            for half in range(2):
                def ev_z(b, f):
                    S.op("act", lambda e: e.activation(out=Z[:, f, :], in_=PM[b][:, 0:TT], func=AF.Identity,
                                                       bias=cv(V_BO + f), scale=1.0),
                         [("pm", b), ("cvec",)], pk(P_ZX, f * 256, (f + 1) * 256))
                    S.op("dve", lambda e: e.scalar_tensor_tensor(
                        out=Z[:, f, :], in0=XT32[:, f, :], scalar=ALPHA, in1=Z[:, f, :],
                        op0=ALU.mult, op1=ALU.add),
                        pk(P_XT32, f * 256, (f + 1) * 256) + pk(P_ZX, f * 256, (f + 1) * 256),
                        pk(P_ZX, f * 256, (f + 1) * 256))
                fm_block("out", 512 * half, R_MR, ev_z)
            ln_stats(P_ZX, 0)
            X1T = fmv(P_X1TX)
            for c in range(8):
                S.op("act", lambda e, c=c: e.activation(out=X1T[:, c, :], in_=Z[:, c, :], func=AF.Identity,
                                                        bias=cv(V_L1B + c), scale=cv(V_L1G + c)),
                     pk(P_ZX, c * 256, (c + 1) * 256) + [("cvec",)], pk(P_X1TX, c * 256, (c + 1) * 256))

            stage("x1t", it, [("x1t", X1T, pk(P_X1TX))])
            yield "tailA"
            for s in range(NSUB):
                for k in range(8):
                    S.op("pe", lambda e, s=s, k=k: e.matmul(PS_[:, 0:72], X1T[:, k, s * 128:(s + 1) * 128],
                                                            WRT[:, k, :], start=(k == 0), stop=(k == 7)),
                         pk(P_X1TX) + [("wrt",)], [("pstat",)])
                S.op("dve", lambda e, s=s: e.tensor_tensor(out=LG[:, s, :], in0=PS_[:, 0:72], in1=BREP[:, :],
                                                           op=ALU.add),
                     [("pstat",), ("brep",)], [("lg", s)])
            X1K = slot(P_X1KX).rearrange("p (s d) -> p s d", s=NSUB)
            for s in range(NSUB):
                for cg in range(2):
                    for cc in range(4):
                        c = cg * 4 + cc
                        S.op("pe", lambda e, s=s, c=c, cc=cc: e.transpose(
                            PO[:, cc * 128:(cc + 1) * 128], X1T[:, c, s * 128:(s + 1) * 128], IDENT),
                            pk(P_X1TX) + [("const",)], [("po",)])
                    S.op("act", lambda e, s=s, cg=cg: e.activation(
                        out=X1K[:, s, cg * 512:(cg + 1) * 512], in_=PO[:, :], func=AF.Copy),
                        [("po",)], pk(P_X1KX, s * 1024 + cg * 512, s * 1024 + (cg + 1) * 512))
            S.dma("sp", lambda e, t0=t0: e.dma_start(
                out=x1d[t0:t0 + TT, :].rearrange("(s p) d -> p s d", p=128), in_=X1K),
                pk(P_X1KX), [("x1d", it)], "x1st")

            yield "tailB1"
            for s in range(NSUB):
                js = it * NSUB + s
                rt = R_
                lg = LG[:, s, :]
                gl = LG[:, s, 0:8]
                el3 = LG[:, s, 8:72].rearrange("p (g e) -> p g e", g=8)

                def D_(fn, reads, writes):
                    S.op("dve", fn, [("r", n_) if isinstance(n_, str) else n_ for n_ in reads],
                         [("r", n_) if isinstance(n_, str) else n_ for n_ in writes])

                def A_(fn, reads, writes):
                    S.op("act", fn, [("r", n_) if isinstance(n_, str) else n_ for n_ in reads],
                         [("r", n_) if isinstance(n_, str) else n_ for n_ in writes])
                LGK = ("lg", s)
                D_(lambda e: e.reduce_max(out=rt["gmax"][:, :], in_=gl, axis=AX.X), [LGK], ["gmax"])
                D_(lambda e: e.tensor_scalar(out=rt["ohg"][:, :], in0=gl, scalar1=rt["gmax"][:, 0:1], scalar2=None,
                                             op0=ALU.is_equal), [LGK, "gmax"], ["ohg"])
                D_(lambda e: e.tensor_scalar(out=rt["ngmax"][:, :], in0=rt["gmax"][:, :], scalar1=-1.0,
                                             scalar2=None, op0=ALU.mult), ["gmax"], ["ngmax"])
                A_(lambda e: e.activation(out=rt["ge"][:, :], in_=gl, func=AF.Exp, bias=rt["ngmax"][:, 0:1],
                                          scale=1.0), [LGK, "ngmax"], ["ge"])
                D_(lambda e: e.reduce_sum(out=rt["gsum"][:, :], in_=rt["ge"][:, :], axis=AX.X), ["ge"], ["gsum"])
                D_(lambda e: e.reciprocal(out=rt["gw"][:, :], in_=rt["gsum"][:, :]), ["gsum"], ["gw"])
                t64_3 = rt["t64"][:, :].rearrange("p (g e) -> p g e", g=8)
                D_(lambda e: e.tensor_tensor(out=t64_3, in0=el3,
                                             in1=rt["ohg"][:, :].unsqueeze(2).to_broadcast([128, 8, 8]),
                                             op=ALU.mult), [LGK, "ohg"], ["t64"])
                D_(lambda e: e.tensor_reduce(out=rt["esel"][:, :],
                                             in_=rt["t64"][:, :].rearrange("p (g e) -> p e g", g=8),
                                             axis=AX.X, op=ALU.add), ["t64"], ["esel"])
                D_(lambda e: e.reduce_max(out=rt["m1"][:, :], in_=rt["esel"][:, :], axis=AX.X), ["esel"], ["m1"])
                D_(lambda e: e.tensor_scalar(out=rt["oh1"][:, :], in0=rt["esel"][:, :], scalar1=rt["m1"][:, 0:1],
                                             scalar2=None, op0=ALU.is_equal), ["esel", "m1"], ["oh1"])
                D_(lambda e: e.scalar_tensor_tensor(out=rt["es2"][:, :], in0=rt["oh1"][:, :], scalar=-1e30,
                                                    in1=rt["esel"][:, :], op0=ALU.mult, op1=ALU.add),
                   ["oh1", "esel"], ["es2"])
                D_(lambda e: e.reduce_max(out=rt["m2"][:, :], in_=rt["es2"][:, :], axis=AX.X), ["es2"], ["m2"])
                D_(lambda e: e.tensor_scalar(out=rt["oh2"][:, :], in0=rt["es2"][:, :], scalar1=rt["m2"][:, 0:1],
                                             scalar2=None, op0=ALU.is_equal), ["es2", "m2"], ["oh2"])
                D_(lambda e: e.tensor_tensor(out=rt["d"][:, :], in0=rt["m2"][:, :], in1=rt["m1"][:, :],
                                             op=ALU.subtract), ["m1", "m2"], ["d"])
                A_(lambda e: e.activation(out=rt["e2"][:, :], in_=rt["d"][:, :], func=AF.Exp), ["d"], ["e2"])
                D_(lambda e: e.tensor_scalar(out=rt["den"][:, :], in0=rt["e2"][:, :], scalar1=1.0, scalar2=None,
                                             op0=ALU.add), ["e2"], ["den"])
                D_(lambda e: e.reciprocal(out=rt["p1"][:, :], in_=rt["den"][:, :]), ["den"], ["p1"])
                D_(lambda e: e.tensor_tensor(out=rt["p2"][:, :], in0=rt["e2"][:, :], in1=rt["p1"][:, :],
                                             op=ALU.mult), ["e2", "p1"], ["p2"])
                for nm, oh in (("oha", "oh1"), ("ohb", "oh2")):
                    D_(lambda e, nm=nm, oh=oh: e.tensor_tensor(
                        out=rt[nm][:, :].rearrange("p (g e) -> p g e", g=8),
                        in0=rt["ohg"][:, :].unsqueeze(2).to_broadcast([128, 8, 8]),
                        in1=rt[oh][:, :].unsqueeze(1).to_broadcast([128, 8, 8]), op=ALU.mult),
                        ["ohg", oh], [nm])
                D_(lambda e: e.tensor_tensor(out=rt["ohs"][:, :], in0=rt["oha"][:, :], in1=rt["ohb"][:, :],
                                             op=ALU.add), ["oha", "ohb"], ["ohs"])
                S.op("pe", lambda e: e.matmul(PS_[:, 128:192], TRIS, rt["ohs"][:, :], start=True, stop=False),
                     [("const",), ("r", "ohs")], [("pstat",)])
                S.op("pe", lambda e: e.matmul(PS_[:, 128:192], ONES[:, :], RUN[:, :], start=False, stop=True),
                     [("ones",), ("run",)], [("pstat",)])
                D_(lambda e: e.tensor_copy(out=rt["cnt"][:, :], in_=PS_[:, 128:192]), [("pstat",)], ["cnt"])
                D_(lambda e: e.tensor_tensor(out=RUN[:, :], in0=RUN[:, :], in1=rt["ohs"][:, :], op=ALU.add),
                   [("run",), "ohs"], [("run",)])
                for a, nm in enumerate(("oha", "ohb")):
                    D_(lambda e, nm=nm: e.tensor_tensor(out=rt["t64"][:, :], in0=rt["cnt"][:, :],
                                                        in1=rt[nm][:, :], op=ALU.mult), ["cnt", nm], ["t64"])
                    D_(lambda e, a=a: e.reduce_sum(out=rt["pos"][:, a:a + 1], in_=rt["t64"][:, :], axis=AX.X),
                       ["t64", "pos"], ["pos"])
                    D_(lambda e, nm=nm: e.tensor_tensor(out=rt["t64"][:, :], in0=IOTA, in1=rt[nm][:, :],
                                                        op=ALU.mult), [("const",), nm], ["t64"])
                    D_(lambda e, a=a: e.reduce_sum(out=rt["eid"][:, a:a + 1], in_=rt["t64"][:, :], axis=AX.X),
                       ["t64", "eid"], ["eid"])
                D_(lambda e: e.tensor_scalar(out=rt["val"][:, :], in0=rt["pos"][:, :], scalar1=float(CAP),
                                             scalar2=None, op0=ALU.is_lt), ["pos"], ["val"])
                D_(lambda e: e.scalar_tensor_tensor(out=rt["slot"][:, :], in0=rt["eid"][:, :], scalar=float(CAP),
                                                    in1=rt["pos"][:, :], op0=ALU.mult, op1=ALU.add),
                   ["eid", "pos"], ["slot"])
                D_(lambda e: e.scalar_tensor_tensor(out=rt["sidf"][:, :], in0=rt["slot"][:, :], scalar=-1.0e6,
                                                    in1=rt["val"][:, :], op0=ALU.add, op1=ALU.mult),
                   ["slot", "val"], ["sidf"])
                D_(lambda e: e.tensor_scalar(out=rt["sidf"][:, :], in0=rt["sidf"][:, :], scalar1=1.0e6,
                                             scalar2=None, op0=ALU.add), ["sidf"], ["sidf"])
                D_(lambda e: e.tensor_tensor(out=rt["tmp2"][:, :], in0=rt["pos"][:, :], in1=rt["val"][:, :],
                                             op=ALU.mult), ["pos", "val"], ["tmp2"])
                D_(lambda e: e.scalar_tensor_tensor(out=rt["gidf"][:, :], in0=rt["eid"][:, :], scalar=float(CAP),
                                                    in1=rt["tmp2"][:, :], op0=ALU.mult, op1=ALU.add),
                   ["eid", "tmp2"], ["gidf"])
                D_(lambda e, js=js: e.tensor_copy(out=SIDX[:, js, :], in_=rt["sidf"][:, :]), ["sidf"],
                   [("sidx", js)])
                D_(lambda e, js=js: e.tensor_copy(out=GIDX[:, js, :], in_=rt["gidf"][:, :]), ["gidf"],
                   [("gidx", js)])
                for a, pn in enumerate(("p1", "p2")):
                    D_(lambda e, a=a, pn=pn, js=js: e.scalar_tensor_tensor(
                        out=WAB[:, js, a:a + 1], in0=rt[pn][:, :], scalar=rt["gw"][:, 0:1],
                        in1=rt["val"][:, a:a + 1], op0=ALU.mult, op1=ALU.mult),
                        [pn, "gw", "val", ("wab", js)], [("wab", js)])
                if DEBUG:
                    D_(lambda e, js=js: e.tensor_copy(out=RTF[:, js, 0:2], in_=rt["gidf"][:, :]),
                       ["gidf", ("rtf",)], [("rtf",)])
                    D_(lambda e, js=js: e.tensor_copy(out=RTF[:, js, 2:4], in_=WAB[:, js, :]),
                       [("wab", js), ("rtf",)], [("rtf",)])
                for a in range(2):
                    S.dma("pool", lambda e, js=js, a=a, s=s: e.indirect_dma_start(
                        out=xs_d, out_offset=bass.IndirectOffsetOnAxis(ap=SIDX[:, js, a:a + 1], axis=0),
                        in_=X1K[:, s, :], in_offset=None, bounds_check=NE * CAP - 1, oob_is_err=False),
                        [("sidx", js)] + pk(P_X1KX, s * 1024, (s + 1) * 1024), [("xs",)], f"sc{a}")

        def phase23():
            def ew(setid, which):
                if setid == 0:
                    return WBT[which][:, :], [("WB", which)]
                return RPOOL[:, which * 2 * SL:(which + 1) * 2 * SL], rk(2 * which) + rk(2 * which + 1)

            XSEs = [slot(0, 0, 1024), slot(0, 1024, 2048)]
            K_XSEs = [pk(0, 0, 1024), pk(0, 1024, 2048)]
            YEs = [slot(1, 0, 1024), slot(1, 1024, 2048)]
            K_YEs = [pk(1, 0, 1024), pk(1, 1024, 2048)]
            HDs = [slot(2, 0, 512), slot(2, 512, 1024)]
            K_HDs = [pk(2, 0, 512), pk(2, 512, 1024)]
            XST = rslot(R_X, 0, 1024).rearrange("p (k t) -> p k t", k=8)
            HDT = rslot(R_X, 1024, 1536).rearrange("p (k t) -> p k t", k=4)
            K_XST, K_HDT = rk(R_X, 0, 1024), rk(R_X, 1024, 1536)

            def load_expert(E):
                st_ = E % 2
                for which, (src, shp) in enumerate(((w1, 8), (w3, 8), (w2, 4))):
                    buf, keys = ew(st_, which)
                    dst = buf.rearrange("p (k n) -> p k n", k=shp)
                    S.dma("pool", lambda e, dst=dst, src=src, E=E: e.dma_start(
                        out=dst, in_=src[E].rearrange("(k p) n -> p k n", p=128)), [], keys, f"ew{st_}{which}")

            def load_xse(E):
                par = E % 2
                S.dma("sp", lambda e, E=E, par=par: e.dma_start(out=XSEs[par], in_=xs_d[E * CAP:(E + 1) * CAP, :]),
                      [("xs",)], K_XSEs[par], f"xse{par}")

            def stage_A(E):
                par = E % 2
                XSE = XSEs[par]
                for cg in range(2):
                    for cc in range(4):
                        c = cg * 4 + cc
                        S.op("pe", lambda e, c=c, cc=cc, XSE=XSE: e.transpose(
                            PO[:, cc * 128:(cc + 1) * 128], XSE[:, c * 128:(c + 1) * 128], IDENT),
                            K_XSEs[par] + [("const",)], [("po",)])
                    S.op("act", lambda e, cg=cg: e.activation(
                        out=XST[:, cg * 4:cg * 4 + 4, :], in_=PO[:, :].rearrange("p (c t) -> p c t", c=4),
                        func=AF.Copy), [("po",)], K_XST)
                if E + 2 < NE:
                    load_xse(E + 2)

            def stage_B(E):
                st_ = E % 2
                W1, k1 = ew(st_, 0)
                W3, k3 = ew(st_, 1)
                W1 = W1.rearrange("p (k n) -> p k n", k=8)
                W3 = W3.rearrange("p (k n) -> p k n", k=8)
                HD = HDs[st_]
                b1 = next_pm()
                b3 = next_pm()
                for (b, W, kk) in ((b1, W1, k1), (b3, W3, k3)):
                    for k in range(8):
                        S.op("pe", lambda e, b=b, W=W, k=k: e.matmul(PM[b][:, :], XST[:, k, :], W[:, k, :],
                                                                     start=(k == 0), stop=(k == 7)),
                             K_XST + kk, [("pm", b)])
                S.op("act", lambda e, b1=b1, HD=HD: e.activation(out=HD, in_=PM[b1][:, :], func=AF.Silu),
                     [("pm", b1)], K_HDs[st_])
                S.op("dve", lambda e, b3=b3, HD=HD: e.tensor_tensor(out=HD, in0=HD, in1=PM[b3][:, :], op=ALU.mult),
                     [("pm", b3)] + K_HDs[st_], K_HDs[st_])

            def stage_C(E):
                st_ = E % 2
                HD = HDs[st_]
                for j in range(4):
                    S.op("pe", lambda e, j=j, HD=HD: e.transpose(PO[:, j * 128:(j + 1) * 128],
                                                                 HD[:, j * 128:(j + 1) * 128], IDENT),
                         K_HDs[st_] + [("const",)], [("po",)])
                S.op("act", lambda e: e.activation(out=HDT, in_=PO[:, :].rearrange("p (c t) -> p c t", c=4),
                                                   func=AF.Copy), [("po",)], K_HDT)

            def stage_D(E):
                st_ = E % 2
                W2, k2 = ew(st_, 2)
                W2 = W2.rearrange("p (k n) -> p k n", k=4)
                YE = YEs[st_]
                for half in range(2):
                    b = next_pm()
                    for j in range(4):
                        S.op("pe", lambda e, b=b, j=j, half=half, W2=W2: e.matmul(
                            PM[b][:, :], HDT[:, j, :], W2[:, j, half * 512:(half + 1) * 512],
                            start=(j == 0), stop=(j == 3)), K_HDT + k2, [("pm", b)])
                    if half == 0:
                        S.op("act", lambda e, b=b, YE=YE: e.activation(out=YE[:, 0:512], in_=PM[b][:, :],
                                                                       func=AF.Copy),
                             [("pm", b)], pk(1, st_ * 1024, st_ * 1024 + 512))
                    else:
                        S.op("dve", lambda e, b=b, YE=YE: e.tensor_copy(out=YE[:, 512:1024], in_=PM[b][:, :]),
                             [("pm", b)], pk(1, st_ * 1024 + 512, st_ * 1024 + 1024))
                S.dma("sp", lambda e, E=E, YE=YE: e.dma_start(out=ys_d[E * CAP:(E + 1) * CAP, :], in_=YE),
                      K_YEs[st_], [("ys",)], f"yst{st_}")
                if E + 2 < NE:
                    load_expert(E + 2)

            load_expert(0)
            load_expert(1)
            load_xse(0)
            load_xse(1)
            stage_A(0)
            stage_B(0)
            stage_A(1)
            for E in range(NE):
                stage_C(E)
                if E + 1 < NE:
                    stage_B(E + 1)
                stage_D(E)
                if E + 2 < NE:
                    stage_A(E + 2)

            G2B2 = slot(2)
            S.dma("sp", lambda e: e.dma_start(out=G2B2, in_=g2b2_d), [], pk(2), "c0")
            if DEBUG:
                S.dma("sp", lambda e: e.dma_start(out=rtd, in_=RTF[:, :, :].rearrange("p a b -> p (a b)")),
                      [("rtf",)], [("rtd",)], "c1")
            out_evs = []
            NPAR = 4
            PBASE = (3 * SL, 3 * SL + 3072, 3 * SL + 6144, 0)

            def pkabs(lo, hi):
                return [("P", w // SL, (w % SL) // 256) for w in range(lo - lo % 256, hi, 256)]

            def bufs(js):
                b0 = PBASE[js % NPAR]
                YA, YB, X1 = POOL[:, b0:b0 + 1024], POOL[:, b0 + 1024:b0 + 2048], POOL[:, b0 + 2048:b0 + 3072]
                return YA, YB, X1, pkabs(b0, b0 + 1024), pkabs(b0 + 1024, b0 + 2048), pkabs(b0 + 2048, b0 + 3072)

            def stv(js):
                return {n: L2S[:, js, i_:i_ + 1] for i_, n in enumerate(("s1", "nmu", "vs", "sd", "rstd"))}

            def p3_load(js):
                par = js % NPAR
                YA, YB, X1, kYA, kYB, kX1 = bufs(js)
                S.dma("pool", lambda e: e.indirect_dma_start(
                    out=YA, out_offset=None, in_=ys_d,
                    in_offset=bass.IndirectOffsetOnAxis(ap=GIDX[:, js, 0:1], axis=0)),
                    [("ys",), ("gidx", js)], kYA, f"ga{par}")
                S.dma("pool", lambda e: e.indirect_dma_start(
                    out=YB, out_offset=None, in_=ys_d,
                    in_offset=bass.IndirectOffsetOnAxis(ap=GIDX[:, js, 1:2], axis=0)),
                    [("ys",), ("gidx", js)], kYB, f"gb{par}")
                S.dma("sp", lambda e: e.dma_start(out=X1, in_=x1d[js * 128:(js + 1) * 128, :]),
                      [("x1d", js // NSUB)], kX1, f"x1l{par}")

            def p3_s1(js):
                YA, YB, X1, kYA, kYB, kX1 = bufs(js)
                st = stv(js)
                S.op("act", lambda e: e.activation(out=YA, in_=YA, func=AF.Copy, scale=WAB[:, js, 0:1]),
                     kYA + [("wab", js)], kYA)
                S.op("dve", lambda e: e.scalar_tensor_tensor(
                    out=YB, in0=YB, scalar=WAB[:, js, 1:2], in1=YA, op0=ALU.mult, op1=ALU.add),
                    kYB + kYA + [("wab", js)], kYB)
                S.op("dve", lambda e: e.scalar_tensor_tensor(
                    out=YB, in0=X1, scalar=ALPHA, in1=YB, op0=ALU.mult, op1=ALU.add), kX1 + kYB, kYB)
                S.op("dve", lambda e: e.reduce_sum(out=st["s1"], in_=YB, axis=AX.X), kYB, [("l2", js, "s1")])
                S.op("dve", lambda e: e.tensor_scalar(out=st["nmu"], in0=st["s1"], scalar1=-1.0 / D,
                                                      scalar2=None, op0=ALU.mult),
                     [("l2", js, "s1")], [("l2", js, "nmu")])
                S.op("act", lambda e: e.activation(out=YA, in_=YB, func=AF.Square, bias=st["nmu"], scale=1.0),
                     kYB + kYA + [("l2", js, "nmu")], kYA)

            def p3_s2(js):
                YA, YB, X1, kYA, kYB, kX1 = bufs(js)
                st = stv(js)
                S.op("dve", lambda e: e.reduce_sum(out=st["vs"], in_=YA, axis=AX.X), kYA, [("l2", js, "vs")])
                S.op("act", lambda e: e.activation(out=st["sd"], in_=st["vs"], func=AF.Sqrt,
                                                   bias=EPS[:, 0:1], scale=1.0 / D),
                     [("l2", js, "vs"), ("eps",)], [("l2", js, "sd")])

            def p3_s3(js):
                par = js % NPAR
                YA, YB, X1, kYA, kYB, kX1 = bufs(js)
                st = stv(js)
                S.op("dve", lambda e: e.reciprocal(out=st["rstd"], in_=st["sd"]),
                     [("l2", js, "sd")], [("l2", js, "rstd")])
                S.op("dve", lambda e: e.scalar_tensor_tensor(
                    out=YB, in0=YB, scalar=st["nmu"], in1=G2B2[:, 0:D], op0=ALU.add, op1=ALU.mult),
                    kYB + [("l2", js, "nmu")] + pk(2), kYB)
                S.op("dve", lambda e: e.scalar_tensor_tensor(
                    out=YB, in0=YB, scalar=st["rstd"], in1=G2B2[:, D:2 * D], op0=ALU.mult, op1=ALU.add),
                    kYB + [("l2", js, "rstd")] + pk(2), kYB)
                ev = S.dma("sp", lambda e: e.dma_start(out=out[js * 128:(js + 1) * 128, :], in_=YB),
                           kYB, [("out", js)], f"ost{par}")
                out_evs.append(ev)

            for js in range(NPAR):
                p3_load(js)
            for step in range(16 + 2):
                if step < 16:
                    p3_s1(step)
                if 0 <= step - 1 < 16:
                    p3_s2(step - 1)
                if 0 <= step - 2 < 16:
                    p3_s3(step - 2)
                    if step - 2 + NPAR < 16:
                        p3_load(step - 2 + NPAR)
            for ev in out_evs:
                S.wait_event("sp", ev)
            if DEBUG:
                for k_ in ("x1st", "c1"):
                    ent = S.dsem[k_]
                    S.wait_event("sp", ("d_" + k_, ent[0], 16 * ent[1], None))


        try:
            gens = [phase1_tile(it) for it in range(NTILE)]
            next(gens[0])
            next(gens[0])
            for it in range(NTILE):
                next(gens[it])
                next(gens[it])
                if it + 1 < NTILE:
                    next(gens[it + 1])
                for _ in gens[it]:
                    pass
                if it + 1 < NTILE:
                    next(gens[it + 1])
            phase23()
        except _Stop:
            for ev in dbg_evs:
                S.wait_event("pool", ev)

        block = es.enter_context(nc.Block())
        S.emit_all(block)
    return nc


def host_prep(inp):
    f = lambda a: np.ascontiguousarray(np.asarray(a, dtype=np.float32))
    b_in = f(inp["b_in"])[0]

    def colvec(v):
        return v.reshape(-1, 128).T

    cvec = np.zeros((128, NVEC), np.float32)
    cvec[:, V_BA:V_BA + 8] = colvec(b_in[C_A:C_A + 1024])
    cvec[:, V_BG:V_BG + 8] = colvec(b_in[C_G:C_G + 1024])
    cvec[:, V_BQ:V_BQ + 4] = colvec(b_in[C_Q:C_Q + 512])
    cvec[:, V_BR:V_BR + 8] = colvec(b_in[C_R:C_R + 1024])
    cvec[:, V_BGA:V_BGA + 8] = colvec(b_in[C_GA:C_GA + 1024])
    cvec[:, V_BGB:V_BGB + 8] = colvec(b_in[C_GB:C_GB + 1024])
    cvec[0:16, V_BF] = b_in[C_F:C_F + 16]
    cvec[:, V_CB:V_CB + 8] = colvec(f(inp["conv_b"])[0])
    cvec[:, V_CLG:V_CLG + 8] = colvec(f(inp["conv_ln_g"])[0])
    cvec[:, V_CLB:V_CLB + 8] = colvec(f(inp["conv_ln_b"])[0])
    cvec[:, V_BCO:V_BCO + 8] = colvec(f(inp["b_conv_out"])[0])
    cvec[:, V_NG:V_NG + 8] = colvec(f(inp["gla_norm_g"])[0].reshape(-1))
    cvec[:, V_BO:V_BO + 8] = colvec(f(inp["b_out"])[0])
    cvec[:, V_L1G:V_L1G + 8] = colvec(f(inp["ln1_g"])[0])
    cvec[:, V_L1B:V_L1B + 8] = colvec(f(inp["ln1_b"])[0])
    cw = f(inp["conv_w"])[0]
    cvec[:, V_CW:V_CW + 248] = cw.T.reshape(8, 128, 31).transpose(1, 0, 2).reshape(128, 248)

    consts = np.zeros((128, NCONST), np.float32)
    consts[:, K_ID:K_ID + 128] = np.eye(128, dtype=np.float32)
    sp = np.arange(128)[:, None]
    tt = np.arange(128)[None, :]
    consts[:, K_TRIU:K_TRIU + 128] = ((sp // 64 == tt // 64) & (sp > tt)).astype(np.float32)
    consts[:, K_TRIS:K_TRIS + 128] = (sp < tt).astype(np.float32)
    consts[:, K_IND:K_IND + 2] = (sp // 64 == np.arange(2)[None, :]).astype(np.float32)
    consts[:, K_IOTA:K_IOTA + 64] = np.arange(64, dtype=np.float32)[None, :]

    brow = np.zeros((4, 512), np.float32)
    brow[0] = b_in[C_K:C_K + 512]
    brow[1] = b_in[C_V:C_V + 512]
    brow[2] = b_in[C_V + 512:C_V + 1024]
    brow[3] = f(inp["b_gate_up"])[0]
    brep = np.tile(np.concatenate([f(inp["b_router_group"])[0], f(inp["b_router_expert"])[0]])[None, :], (128, 1))
    g2b2 = np.tile(np.concatenate([f(inp["ln2_g"])[0], f(inp["ln2_b"])[0]])[None, :], (128, 1))
    w_r = np.concatenate([f(inp["w_router_group"])[0], f(inp["w_router_expert"])[0]], axis=1)
    shared = {
        "w_in": f(inp["w_in"])[0], "w_co": f(inp["w_conv_out"])[0], "w_gla": f(inp["w_gla_out"])[0],
        "w_out": f(inp["w_out"])[0], "w_gu": f(inp["w_gate_up"])[0], "w_r": np.ascontiguousarray(w_r),
        "w1": f(inp["w1"])[0], "w3": f(inp["w3"])[0], "w2": f(inp["w2"])[0],
        "cvec": cvec, "consts": consts, "brow": brow, "brep": np.ascontiguousarray(brep),
        "g2b2": np.ascontiguousarray(g2b2),
    }
    xs = f(inp["x"])
    return [dict(shared, x=np.ascontiguousarray(xs[b])) for b in range(NCORES)]


_NC_CACHE = {}


def kernel(**inputs):
    in_maps = host_prep(inputs)
    if "nc" not in _NC_CACHE:
        _NC_CACHE["nc"] = build_nc()
    nc = _NC_CACHE["nc"]
    res = run_bass_kernel_spmd(nc, in_maps, core_ids=list(range(NCORES)))
    kernel.last_results = res
    return np.stack([np.asarray(r["out"], dtype=np.float32) for r in res.results], axis=0)
```

```python
import contextlib
import numpy as np
import concourse.bass as bass
import concourse.mybir as mybir
from concourse.bass_utils import run_bass_kernel_spmd

F32 = mybir.dt.float32
F32R = mybir.dt.float32r
I32 = mybir.dt.int32
AF = mybir.ActivationFunctionType
ALU = mybir.AluOpType
AX = mybir.AxisListType

NCORES = 8
T = 2048
D = 1024
TT = 256
NTILE = T // TT
NSUB = TT // 128
NCHK = TT // 64
DIN = 7184
NE = 64
CAP = 128
ALPHA = float((2.0 * 1) ** 0.25)
LN_EPS = 1e-5
RMS_EPS = 1e-6
SAME_ENGINE_SYNC = True
DEBUG = False
DBG_STAGE = None
DBG_COLS = 16384


class _Stop(Exception):
    pass

C_A, C_G, C_Q, C_K, C_V, C_R, C_F, C_GA, C_GB = 0, 1024, 2048, 2560, 3072, 4096, 5120, 5136, 6160

V_BA, V_BG, V_BQ, V_BR, V_BGA, V_BGB = 0, 8, 16, 20, 28, 36
V_BF = 44
V_CB, V_CLG, V_CLB, V_BCO, V_NG, V_BO, V_L1G, V_L1B = 45, 53, 61, 69, 77, 85, 93, 101
V_CW = 109
NVEC = V_CW + 8 * 31
K_ID, K_TRIU, K_TRIS, K_IND, K_IOTA = 0, 128, 256, 384, 386
NCONST = 386 + 64


class _Rec:
    def __getattr__(self, name):
        return lambda *a, **k: (name, a, k)


_REC = _Rec()


class Sched:
    def __init__(self, nc, es):
        self.nc = nc
        self.es = es
        self.order = ["pe", "act", "dve", "pool", "sp"]
        self.prog = {k: [] for k in self.order}
        self.esem = {k: es.enter_context(nc.semaphore("sem_" + k)) for k in ("pe", "act", "dve", "pool")}
        self.ecnt = {k: 0 for k in self.esem}
        self.dsem = {}
        self.seen = {k: {} for k in self.order}
        self.lastw = {}
        self.readers = {}
        self.nwaits = 0

    def _collect(self, eng, reads, writes):
        evs = []
        for k in reads:
            w = self.lastw.get(k)
            if w is not None:
                evs.append(w)
        for k in writes:
            w = self.lastw.get(k)
            if w is not None:
                evs.append(w)
            evs.extend(self.readers.get(k, ()))
        best = {}
        seen = self.seen[eng]
        for (sname, sem, val, e_eng) in evs:
            if e_eng == eng and (eng == "pe" or not SAME_ENGINE_SYNC):
                continue
            if seen.get(sname, 0) >= val:
                continue
            if sname not in best or best[sname][1] < val:
                best[sname] = (sem, val)
        waits = []
        for sname, (sem, val) in best.items():
            seen[sname] = val
            waits.append((sem, val))
        self.nwaits += len(waits)
        return waits

    def _commit(self, ev, reads, writes):
        for k in reads:
            self.readers.setdefault(k, []).append(ev)
        for k in writes:
            self.lastw[k] = ev
            self.readers[k] = []

    PSUM_KEYS = ("pm", "pk", "po", "pstat")

    def op(self, eng, fn, reads=(), writes=()):
        writes = list(writes) + [k for k in reads if k[0] in self.PSUM_KEYS]
        reads = [k for k in reads if k[0] not in self.PSUM_KEYS]
        waits = self._collect(eng, reads, writes)
        self.ecnt[eng] += 1
        val = self.ecnt[eng]
        sem = self.esem[eng]
        rec = fn(_REC)

        def emit(e, waits=waits, rec=rec, sem=sem):
            for (s, v) in waits:
                e.wait_ge(s, v)
            getattr(e, rec[0])(*rec[1], **rec[2]).then_inc(sem, 1)

        self.prog[eng].append(emit)
        ev = ("e_" + eng, sem, val, eng)
        self._commit(ev, reads, writes)
        return ev

    def dma(self, q, fn, reads, writes, semkey):
        reads = list(reads)
        writes = list(writes)
        if semkey not in self.dsem:
            self.dsem[semkey] = [self.es.enter_context(self.nc.semaphore("d_" + semkey)), 0]
        ent = self.dsem[semkey]
        sname = "d_" + semkey
        waits = self._collect(q, reads, writes)
        if ent[1] > 0 and self.seen[q].get(sname, 0) < 16 * ent[1]:
            waits = [w for w in waits if w[0] is not ent[0]]
            waits.append((ent[0], 16 * ent[1]))
            self.seen[q][sname] = 16 * ent[1]
        ent[1] += 1
        val = 16 * ent[1]
        sem = ent[0]
        rec = fn(_REC)

        def emit(e, waits=waits, rec=rec, sem=sem):
            for (s, v) in waits:
                e.wait_ge(s, v)
            getattr(e, rec[0])(*rec[1], **rec[2]).then_inc(sem, 16)

        self.prog[q].append(emit)
        ev = (sname, sem, val, None)
        self._commit(ev, reads, writes)
        return ev

    def wait_event(self, eng, ev):
        (sname, sem, val, _) = ev
        if self.seen[eng].get(sname, 0) >= val:
            return
        self.seen[eng][sname] = val

        def emit(e, sem=sem, val=val):
            e.wait_ge(sem, val)

        self.prog[eng].append(emit)

    def emit_all(self, block):
        progs = self.prog

        @block.tensor
        def _(e):
            for f in progs["pe"]:
                f(e)

        @block.scalar
        def _(e):
            for f in progs["act"]:
                f(e)

        @block.vector
        def _(e):
            for f in progs["dve"]:
                f(e)

        @block.gpsimd
        def _(e):
            for f in progs["pool"]:
                f(e)

        @block.sync
        def _(e):
            for f in progs["sp"]:
                f(e)


def build_nc():
    nc = bass.Bass("TRN2", target_bir_lowering=False)

    def din(name, shape, dt=F32):
        return nc.dram_tensor(name, list(shape), dt, kind="ExternalInput").ap()

    x = din("x", [T, D])
    w_in = din("w_in", [D, DIN])
    w_co = din("w_co", [D, D])
    w_gla = din("w_gla", [D, D])
    w_out = din("w_out", [D, D])
    w_gu = din("w_gu", [16, 512])
    w_r = din("w_r", [D, 72])
    w1 = din("w1", [NE, D, 512])
    w3 = din("w3", [NE, D, 512])
    w2 = din("w2", [NE, 512, D])
    cvec_d = din("cvec", [128, NVEC])
    const_d = din("consts", [128, NCONST])
    brow_d = din("brow", [4, 512])
    brep_d = din("brep", [128, 72])
    g2b2_d = din("g2b2", [128, 2 * D])
    out = nc.dram_tensor("out", [T, D], F32, kind="ExternalOutput").ap()
    dbg_kind = "ExternalOutput" if DEBUG else "Internal"
    x1d = nc.dram_tensor("x1d", [T, D], F32, kind=dbg_kind).ap()
    rtd = nc.dram_tensor("rtd", [128, 16 * 4], F32, kind=dbg_kind).ap()
    nrows_scr = 128 if DBG_STAGE else NE * CAP
    xs_d = nc.dram_tensor("xs_d", [nrows_scr, D], F32, kind="Internal").ap()
    dbg_d = nc.dram_tensor("dbg", [128, DBG_COLS], F32, kind="ExternalOutput").ap() if DBG_STAGE else None
    dbg_layout = {}
    nc.dbg_layout = dbg_layout
    ys_d = nc.dram_tensor("ys_d", [nrows_scr, D], F32, kind="Internal").ap()

    with contextlib.ExitStack() as es:
        S = Sched(nc, es)

        def sb(name, shape, dt=F32):
            return es.enter_context(nc.sbuf_tensor(name, list(shape), dt))

        def ps(name, shape):
            return es.enter_context(nc.psum_tensor(name, list(shape), F32))

        NSLOT = 8
        SL = 2048
        POOL = sb("pool", [128, NSLOT * SL])
        RPOOL = sb("rpool", [128, 7 * SL], F32R)
        WBT = [sb(f"wb{r}", [128, 8 * 512], F32R) for r in range(3)]
        UT = sb("ut", [128, 8, TT + 30])
        CVEC = sb("cvec_t", [128, NVEC])
        CONST = sb("const_t", [128, NCONST])
        ONES = sb("ones", [128, 128])
        ONESR = sb("onesr", [128, 128], F32R)
        BREP = sb("brep_t", [128, 72])
        WRT = sb("wr_t", [128, 8, 72])
        WGU = sb("wgu_t", [32, 512])
        WFL = sb("wfl_t", [128, 8, 16])
        EPS = sb("eps_t", [128, 2])
        MU = sb("mu", [128, TT])
        MSQ = sb("msq", [128, TT])
        SD = MSQ
        RSTD = MSQ
        DEC = sb("dec", [128, NSUB * 4 * 2])
        FLT = sb("flt", [32, TT])
        L2S = sb("l2s", [128, 16, 8])
        ST = [sb(f"st{i}", [128, 4, 256]) for i in range(2)]
        LG = sb("lg", [128, NSUB, 72])
        RUN = sb("run", [128, 64])
        GIDX = sb("gidx", [128, 16, 2], I32)
        SIDX = sb("sidx", [128, 16, 2], I32)
        WAB = sb("wab", [128, 16, 2])
        RTF = sb("rtf", [128, 16, 4]) if DEBUG else None
        R_ = {n: sb("r_" + n, [128, w]) for n, w in [
            ("gmax", 1), ("ngmax", 1), ("ohg", 8), ("ge", 8), ("gsum", 1), ("gw", 1), ("t64", 64), ("esel", 8),
            ("m1", 1), ("oh1", 8), ("es2", 8), ("m2", 1), ("oh2", 8), ("d", 1), ("e2", 1), ("den", 1), ("p1", 1),
            ("p2", 1), ("oha", 64), ("ohb", 64), ("ohs", 64), ("cnt", 64), ("pos", 2), ("eid", 2), ("val", 2),
            ("slot", 2), ("sidf", 2), ("gidf", 2), ("tmp2", 2)]}

        PM = [ps(f"pm{i}", [128, 512]) for i in range(4)]
        PK = ps("pk", [128, 1024])
        PO = ps("po", [128, 512])
        PS_ = ps("pstat", [128, 512])

        def slot(i, lo=0, hi=SL):
            return POOL[:, i * SL + lo:i * SL + hi]

        def fmv(i, dt=None):
            ap = slot(i).rearrange("p (c t) -> p c t", c=8)
            return ap.bitcast(dt) if dt is not None else ap

        def pk(i, lo=0, hi=SL):
            return [("P", i, g) for g in range(lo // 256, (hi + 255) // 256)]

        def wbv(r):
            return WBT[r][:, :].rearrange("p (k n) -> p k n", k=8)

        def rslot(i, lo=0, hi=SL):
            return RPOOL[:, i * SL + lo:i * SL + hi]

        def rmv(i):
            return rslot(i).rearrange("p (c t) -> p c t", c=8)

        def rk(i, lo=0, hi=SL):
            return [("R", i, g) for g in range(lo // 256, (hi + 255) // 256)]

        BROWS = [rslot(6, j * 512, (j + 1) * 512) for j in range(3)]
        SG = slot(3, 0, 512)

        def wbk(r):
            return [("WB", r)]

        def cv(col, n=1):
            return CVEC[:, col:col + n]

        pm_ctr = [0]

        def next_pm():
            i = pm_ctr[0] % 4
            pm_ctr[0] += 1
            return i

        S.dma("sp", lambda e: e.dma_start(out=CVEC[:, :], in_=cvec_d), [], [("cvec",)], "c0")
        S.dma("sp", lambda e: e.dma_start(out=CONST[:, :], in_=const_d), [], [("const",)], "c1")
        S.dma("sp", lambda e: e.dma_start(out=BREP[:, :], in_=brep_d), [], [("brep",)], "c2")
        S.dma("sp", lambda e: e.dma_start(out=WRT[:, :, :], in_=w_r.rearrange("(k p) n -> p k n", p=128)),
              [], [("wrt",)], "c3")
        S.op("dve", lambda e: e.memset(WGU[:, :], 0.0), [], [("wgu",)])
        S.dma("sp", lambda e: e.dma_start(out=WGU[0:16, :], in_=w_gu), [], [("wgu",)], "c0")
        S.dma("sp", lambda e: e.dma_start(out=WFL[:, :, :],
                                          in_=w_in[:, C_F:C_F + 16].rearrange("(k p) n -> p k n", p=128)),
              [], [("wfl",)], "c1")
        S.dma("sp", lambda e: e.dma_start(out=WGU[16:17, :], in_=brow_d[3:4, :]), [("wgu",)], [("wgu",)], "c2")
        S.op("dve", lambda e: e.memset(FLT[:, :], 1.0), [], [("flt",)])
        for j in range(3):
            S.dma("pool", lambda e, j=j: e.dma_start(out=BROWS[j][0:1, :], in_=brow_d[j:j + 1, :]),
                  [], rk(6, j * 512, (j + 1) * 512), "c4")
        S.op("dve", lambda e: e.memset(ONES[:, :], 1.0), [], [("ones",)])
        S.op("dve", lambda e: e.tensor_copy(out=ONESR[:, :], in_=ONES[:, :]), [("ones",)], [("onesr",)])
        S.op("dve", lambda e: e.memset(EPS[:, 0:1], LN_EPS), [], [("eps",)])
        S.op("dve", lambda e: e.memset(EPS[:, 1:2], RMS_EPS), [("eps",)], [("eps",)])
        S.op("dve", lambda e: e.memset(ST[0][:, :, :], 0.0), [], [("st", 0, h) for h in range(4)])
        S.op("dve", lambda e: e.memset(RUN[:, :], 0.0), [], [("run",)])
        S.op("dve", lambda e: e.memset(UT[:, :, 0:30], 0.0), [], [("uhalo",)])
        if not DBG_STAGE:
            S.op("dve", lambda e: e.memset(slot(7), 0.0), [], pk(7))
            S.op("dve", lambda e: e.tensor_copy(out=rslot(4), in_=slot(7)), pk(7), rk(4))
            ZSRC = rslot(4).bitcast(F32)
        zero_next = [0]

        def issue_zero(n_):
            for _ in range(n_):
                g_ = zero_next[0]
                if g_ >= 32 or DBG_STAGE:
                    return
                S.dma("sp", lambda e, g_=g_: e.dma_start(
                    out=xs_d[g_ * 256:(g_ + 1) * 256, :].rearrange("(e p) d -> p e d", p=128),
                    in_=ZSRC.rearrange("p (e d) -> p e d", e=2)), rk(4), [("xs",)], f"z{g_ % 4}")
                zero_next[0] += 1
        IDENT = CONST[:, K_ID:K_ID + 128]
        TRIU = CONST[:, K_TRIU:K_TRIU + 128]
        TRIS = CONST[:, K_TRIS:K_TRIS + 128]
        IND = CONST[:, K_IND:K_IND + 2]
        IOTA = CONST[:, K_IOTA:K_IOTA + 64]

        def wsrc(name, c0):
            m = {"in": w_in, "co": w_co, "gla": w_gla, "out": w_out}[name]
            return m[:, c0:c0 + 512].rearrange("(k p) n -> p k n", p=128)

        blk_list = []
        for it in range(NTILE):
            for nm, c0 in [("in", C_G), ("in", C_G + 512), ("in", C_A), ("in", C_A + 512),
                           ("in", C_K), ("in", C_V), ("in", C_V + 512), ("in", C_Q),
                           ("in", C_GA), ("in", C_GA + 512), ("in", C_R), ("in", C_R + 512),
                           ("in", C_GB), ("in", C_GB + 512), ("gla", 0), ("gla", 512),
                           ("co", 0), ("co", 512),
                           ("out", 0), ("out", 512)]:
                blk_list.append((nm, c0))
        NBLK = len(blk_list)
        blk_issued = [0]
        blk_used = [0]

        def issue_block():
            j = blk_issued[0]
            if j >= NBLK:
                return
            nm, c0 = blk_list[j]
            r = j % 3
            S.dma("pool", lambda e, r=r, nm=nm, c0=c0: e.dma_start(out=wbv(r), in_=wsrc(nm, c0)),
                  [], wbk(r), f"wb{r}")
            blk_issued[0] += 1

        def take_block(nm, c0):
            j = blk_used[0]
            assert blk_list[j] == (nm, c0), (j, blk_list[j], nm, c0)
            blk_used[0] += 1
            return j % 3

        def release_block():
            issue_block()
            issue_zero(4)

        issue_block()
        issue_block()
        issue_block()

        def fm_block(nm, c0, rhs_slot, evac):
            r = take_block(nm, c0)
            W = wbv(r)
            X = rmv(rhs_slot)
            for fc in range(4):
                b = next_pm()
                for k in range(8):
                    S.op("pe", lambda e, b=b, k=k, fc=fc, W=W, X=X: e.matmul(
                        PM[b][:, 0:TT], W[:, k, fc * 128:(fc + 1) * 128], X[:, k, :],
                        start=(k == 0), stop=(k == 7)),
                        wbk(r) + rk(rhs_slot), [("pm", b)])
                evac(b, (c0 % 1024) // 128 + fc)
            release_block()

        def tm_block(nm, c0, bias_j, evac):
            r = take_block(nm, c0)
            W = wbv(r)
            X = rmv(R_XTR)
            for s in range(NSUB):
                b = next_pm()
                for k in range(8):
                    S.op("pe", lambda e, b=b, k=k, s=s, W=W, X=X: e.matmul(
                        PM[b][:, :], X[:, k, s * 128:(s + 1) * 128], W[:, k, :],
                        start=(k == 0), stop=False),
                        wbk(r) + rk(R_XTR), [("pm", b)])
                S.op("pe", lambda e, b=b, j=bias_j: e.matmul(
                    PM[b][:, :], ONESR[0:1, :], BROWS[j][0:1, :],
                    start=False, stop=True), [("onesr",)] + rk(6, bias_j * 512, (bias_j + 1) * 512), [("pm", b)])
                evac(b, s)
            release_block()

        def ln_stats(src, eps_col):
            SRC = fmv(src)
            SQ = rmv(R_SQ)
            S.op("act", lambda e: e.activation(out=rslot(R_SQ), in_=slot(src), func=AF.Square),
                 pk(src), rk(R_SQ))
            for k in range(8):
                S.op("pe", lambda e, k=k: e.matmul(PS_[:, 0:TT], ONES[:, :], SRC[:, k, :],
                                                   start=(k == 0), stop=(k == 7)),
                     [("ones",)] + pk(src), [("pstat",)])
            S.op("act", lambda e: e.activation(out=MU[:, :], in_=PS_[:, 0:TT], func=AF.Copy, scale=1.0 / D),
                 [("pstat",)], [("mu",)])
            for k in range(8):
                S.op("pe", lambda e, k=k: e.matmul(PS_[:, TT:2 * TT], ONESR[:, :], SQ[:, k, :],
                                                   start=(k == 0), stop=(k == 7)),
                     [("onesr",)] + rk(R_SQ), [("pstat",)])
            S.op("dve", lambda e: e.tensor_tensor(out=MSQ[:, :], in0=MU[:, :], in1=MU[:, :], op=ALU.mult),
                 [("mu",)], [("msq",)])
            S.op("dve", lambda e: e.scalar_tensor_tensor(out=MSQ[:, :], in0=PS_[:, TT:2 * TT], scalar=1.0 / D,
                                                         in1=MSQ[:, :], op0=ALU.mult, op1=ALU.subtract),
                 [("pstat",), ("msq",)], [("msq",)])
            S.op("act", lambda e: e.activation(out=SD[:, :], in_=MSQ[:, :], func=AF.Sqrt,
                                               bias=EPS[:, eps_col:eps_col + 1], scale=1.0),
                 [("msq",), ("eps",)], [("msq",)])
            S.op("dve", lambda e: e.reciprocal(out=RSTD[:, :], in_=SD[:, :]), [("msq",)], [("msq",)])
            MUB = MU[:, :].unsqueeze(1).to_broadcast([128, 8, TT])
            RSB = RSTD[:, :].unsqueeze(1).to_broadcast([128, 8, TT])
            S.op("dve", lambda e: e.tensor_tensor(out=SRC, in0=SRC, in1=MUB, op=ALU.subtract),
                 pk(src) + [("mu",)], pk(src))
            S.op("dve", lambda e: e.tensor_tensor(out=SRC, in0=SRC, in1=RSB, op=ALU.mult),
                 pk(src) + [("msq",)], pk(src))

        P_CVX = P_ZX = 0
        P_XT32 = 1
        P_OTX = P_X1TX = 2
        P_G = P_GA = 3
        P_XTOK = P_QT = 4
        P_SRX = 6
        P_GBX = 7
        P_LA = P_M = 5
        P_X1KX = 6
        R_XTR, R_KT, R_V, R_CSR, R_OGR, R_SQ, R_X = 0, 1, 2, 3, 4, 5, 6
        R_MR = R_OGR

        dbg_evs = []

        def stage(name, it, bufs):
            if DBG_STAGE is None or DBG_STAGE != (name, it):
                return
            off = 0
            for (label, ap, keys) in bufs:
                shp = list(ap.shape)
                n = 1
                for d_ in shp[1:]:
                    n *= d_
                npart = shp[0]
                dst = dbg_d[0:npart, off:off + n]
                if len(shp) == 3:
                    dst = dst.rearrange("p (a b) -> p a b", a=shp[1])
                if ap.dtype == F32R:
                    ap = ap.bitcast(F32)
                ev = S.dma("pool", lambda e, dst=dst, ap=ap: e.dma_start(out=dst, in_=ap), keys,
                           [("dbg", label)], "dbg")
                dbg_evs.append(ev)
                dbg_layout[label] = (off, shp)
                off += n
            assert off <= DBG_COLS, off
            raise _Stop()

        def phase1_tile(it):
            t0 = it * TT
            stage("pro", it, [("cvec", CVEC[:, 0:128], [("cvec",)]), ("onesr", ONESR[:, :], [("onesr",)]),
                              ("brow", BROWS[0][0:1, :], rk(6, 0, 512)), ("wfl", WFL[:, :, :], [("wfl",)]),
                              ("ut", UT[:, :, 0:30], [("uhalo",)])])
            XTOK = slot(P_XTOK).rearrange("p (s d) -> p s d", s=NSUB)

            def load_x(tile):
                tt0 = tile * TT
                S.dma("sp", lambda e: e.dma_start(
                    out=XTOK, in_=x[tt0:tt0 + TT, :].rearrange("(s p) d -> p s d", p=128)),
                    [], pk(P_XTOK), "xin")

            if it == 0:
                load_x(0)
            XT32 = fmv(P_XT32)
            XTR = rmv(R_XTR)
            stage("xl", it, [("xtok", XTOK, pk(P_XTOK))])
            for s in range(NSUB):
                for cg in range(2):
                    for cc in range(4):
                        c = cg * 4 + cc
                        S.op("pe", lambda e, s=s, c=c, cc=cc, XTOK=XTOK: e.transpose(
                            PO[:, cc * 128:(cc + 1) * 128], XTOK[:, s, c * 128:(c + 1) * 128], IDENT),
                            pk(P_XTOK) + [("const",)], [("po",)])
                    pov = PO[:, :].rearrange("p (c t) -> p c t", c=4)
                    import os
                    if os.environ.get("XTR_ENG", "act") == "act":
                        S.op("act", lambda e, s=s, cg=cg, pov=pov, XTR=XTR: e.activation(
                            out=XTR[:, cg * 4:cg * 4 + 4, s * 128:(s + 1) * 128], in_=pov, func=AF.Copy),
                            [("po",)], rk(R_XTR))
                    else:
                        S.op("dve", lambda e, s=s, cg=cg, pov=pov, XTR=XTR: e.tensor_copy(
                            out=XTR[:, cg * 4:cg * 4 + 4, s * 128:(s + 1) * 128], in_=pov),
                            [("po",)], rk(R_XTR))
                    if os.environ.get("XTR_ENG", "act") == "act":
                        S.op("dve", lambda e, s=s, cg=cg, pov=pov, XT32=XT32: e.tensor_copy(
                            out=XT32[:, cg * 4:cg * 4 + 4, s * 128:(s + 1) * 128], in_=pov),
                            [("po",)], pk(P_XT32))
                    else:
                        for cc in range(4):
                            S.op("act", lambda e, s=s, cg=cg, cc=cc, XT32=XT32: e.activation(
                                out=XT32[:, cg * 4 + cc, s * 128:(s + 1) * 128], in_=PO[:, cc * 128:(cc + 1) * 128],
                                func=AF.Copy), [("po",)], pk(P_XT32))

            stage("a", it, [("xtr", XTR, rk(R_XTR)), ("xt32", XT32, pk(P_XT32))])
            yield "early1"
            LA = slot(P_LA, 0, 1024).rearrange("p (s n) -> p s n", s=NSUB)
            DK = slot(P_LA, 1024, 2048).rearrange("p (s n) -> p s n", s=NSUB)
            SG2 = slot(P_QT, 1024, 1536)
            K_SG2 = pk(P_QT, 1024, 1536)

            def step_flr():
                for k in range(8):
                    S.op("pe", lambda e, k=k: e.matmul(PS_[0:16, 0:TT], WFL[:, k, :], XT32[:, k, :],
                                                       start=(k == 0), stop=(k == 7)),
                         [("wfl",)] + pk(P_XT32), [("pstat",)])
                S.op("act", lambda e: e.activation(out=FLT[0:16, :], in_=PS_[0:16, 0:TT], func=AF.Identity,
                                                   bias=CVEC[0:16, V_BF:V_BF + 1], scale=1.0),
                     [("pstat",), ("cvec",)], [("flt",)])

            def step_z(s):
                S.op("pe", lambda e: e.matmul(PS_[:, :], FLT[0:17, s * 128:(s + 1) * 128], WGU[0:17, :],
                                              start=True, stop=True),
                     [("flt",), ("wgu",)], [("pstat",)])
                S.op("act", lambda e: e.activation(out=SG2, in_=PS_[:, :], func=AF.Sigmoid),
                     [("pstat",)], K_SG2)
                S.op("act", lambda e: e.activation(out=LA[:, s, :], in_=SG2, func=AF.Ln),
                     K_SG2, pk(P_LA, s * 512, (s + 1) * 512))

            def step_cum(s):
                S.op("pe", lambda e: e.matmul(PS_[:, :], TRIU, LA[:, s, :], start=True, stop=True),
                     [("const",)] + pk(P_LA, s * 512, (s + 1) * 512), [("pstat",)])
                S.op("act", lambda e: e.activation(out=DK[:, s, :], in_=PS_[:, :], func=AF.Exp,
                                                   scale=1.0 / 16.0),
                     [("pstat",)], pk(P_LA, 1024 + s * 512, 1024 + (s + 1) * 512))

            def step_dec():
                for s in range(NSUB):
                    for h in range(4):
                        o = (s * 4 + h) * 2
                        S.op("pe", lambda e, s=s, h=h, o=o: e.matmul(
                            PS_[:, o:o + 2], LA[:, s, h * 128:(h + 1) * 128], IND, start=True, stop=True),
                            [("const",)] + pk(P_LA, s * 512, (s + 1) * 512), [("pstat",)])
                S.op("act", lambda e: e.activation(out=DEC[:, :], in_=PS_[:, 0:NSUB * 8], func=AF.Exp,
                                                   scale=1.0 / 16.0),
                     [("pstat",)], [("dec",)])

            G = fmv(P_G)

            def ev_g(b, f):
                S.op("act", lambda e: e.activation(out=G[:, f, :], in_=PM[b][:, 0:TT], func=AF.Sigmoid,
                                                   bias=cv(V_BG + f), scale=1.0),
                     [("pm", b), ("cvec",)], pk(P_G, f * 256, (f + 1) * 256))

            def ev_a(b, f):
                S.op("dve", lambda e: e.scalar_tensor_tensor(
                    out=UT[:, f, 30:30 + TT], in0=PM[b][:, 0:TT], scalar=cv(V_BA + f), in1=G[:, f, :],
                    op0=ALU.add, op1=ALU.mult),
                    [("pm", b), ("cvec",), ("uhalo",)] + pk(P_G, f * 256, (f + 1) * 256), [("u", f)])

            KT = rslot(R_KT, 1024, 2048).rearrange("p (s n) -> p s n", s=NSUB)
            QT = slot(P_QT, 0, 1024).rearrange("p (h t) -> p h t", h=4)
            VT = rslot(R_V).rearrange("p (s n) -> p s n", s=NSUB)

            def ev_k(b, s):
                S.op("dve", lambda e: e.tensor_tensor(out=KT[:, s, :], in0=PM[b][:, :], in1=DK[:, s, :],
                                                      op=ALU.mult),
                     [("pm", b)] + pk(P_LA, 1024 + s * 512, 1024 + (s + 1) * 512),
                     rk(R_KT, 1024 + s * 512, 1024 + (s + 1) * 512))

            def ev_q(b, f):
                S.op("act", lambda e: e.activation(out=QT[:, f, :], in_=PM[b][:, 0:TT], func=AF.Identity,
                                                   bias=cv(V_BQ + f), scale=1.0),
                     [("pm", b), ("cvec",)], pk(P_QT, f * 256, (f + 1) * 256))

            fm_block("in", C_G, R_XTR, ev_g)
            step_flr()
            fm_block("in", C_G + 512, R_XTR, ev_g)
            step_z(0)
            if it > 0:
                S.op("dve", lambda e: e.tensor_copy(out=UT[:, :, 0:30], in_=UT[:, :, TT:TT + 30]),
                     [("u", c) for c in range(8)] + [("uhalo",)], [("uhalo",)])
            fm_block("in", C_A, R_XTR, ev_a)
            step_z(1)
            fm_block("in", C_A + 512, R_XTR, ev_a)
            step_cum(0)
            step_cum(1)
            step_dec()
            stage("la", it, [("la", LA, pk(P_LA, 0, 1024)), ("dk", DK, pk(P_LA, 1024, 2048)),
                             ("dec", DEC[:, :], [("dec",)]), ("flt", FLT[:, :], [("flt",)])])
            stage("u", it, [("g", G, pk(P_G)), ("ut", UT[:, :, :], [("u", c) for c in range(8)] + [("uhalo",)])])
            tm_block("in", C_K, 0, ev_k)
            for half in range(2):
                def ev_v(b, s, half=half):
                    S.op("act", lambda e: e.activation(out=VT[:, s, half * 512:(half + 1) * 512],
                                                       in_=PM[b][:, :], func=AF.Copy),
                         [("pm", b)], rk(R_V, s * 1024 + half * 512, s * 1024 + (half + 1) * 512))
                tm_block("in", C_V + 512 * half, 1 + half, ev_v)
            fm_block("in", C_Q, R_XTR, ev_q)

            stage("kvq", it, [("kt", KT, rk(R_KT, 1024, 2048)), ("vt", VT, rk(R_V)), ("qt", QT, pk(P_QT, 0, 1024))])
            yield "early"
            GA = fmv(P_GA)
            for half in range(0):
                def ev_ga(b, f, half=half):
                    ff = half * 4 + f % 4
                    S.op("act", lambda e: e.activation(out=GA[:, ff, :], in_=PM[b][:, 0:TT], func=AF.Sigmoid,
                                                       bias=cv(V_BGA + ff), scale=1.0),
                         [("pm", b), ("cvec",)], pk(P_GA, ff * 256, (ff + 1) * 256))
                fm_block("in", C_GA + 512 * half, R_XTR, ev_ga)

            SR = fmv(P_SRX)
            for half in range(0):
                def ev_r(b, f):
                    S.op("act", lambda e: e.activation(out=SR[:, f, :], in_=PM[b][:, 0:TT], func=AF.Silu,
                                                       bias=cv(V_BR + f), scale=1.0),
                         [("pm", b), ("cvec",)], pk(P_SRX, f * 256, (f + 1) * 256))
                fm_block("in", C_R + 512 * half, R_XTR, ev_r)
            GB = fmv(P_GBX)
            for half in range(0):
                def ev_gb(b, f):
                    S.op("act", lambda e: e.activation(out=GB[:, f, :], in_=PM[b][:, 0:TT], func=AF.Sigmoid,
                                                       bias=cv(V_BGB + f), scale=1.0),
                         [("pm", b), ("cvec",)], pk(P_GBX, f * 256, (f + 1) * 256))
                fm_block("in", C_GB + 512 * half, R_XTR, ev_gb)
            CVT = fmv(P_CVX)

            def conv_part(jlo, jhi):
                for j in range(jlo, jhi):
                    for c in range(8):
                        wcol = cv(V_CW + c * 31 + j)
                        if j == 0:
                            S.op("dve", lambda e, c=c, wcol=wcol: e.tensor_scalar(
                                out=CVT[:, c, :], in0=UT[:, c, 0:TT], scalar1=wcol, scalar2=cv(V_CB + c),
                                op0=ALU.mult, op1=ALU.add),
                                [("u", c), ("uhalo",), ("cvec",)], pk(P_CVX, c * 256, (c + 1) * 256))
                        else:
                            S.op("dve", lambda e, c=c, j=j, wcol=wcol: e.scalar_tensor_tensor(
                                out=CVT[:, c, :], in0=UT[:, c, j:j + TT], scalar=wcol, in1=CVT[:, c, :],
                                op0=ALU.mult, op1=ALU.add),
                                [("u", c), ("uhalo",), ("cvec",)] + pk(P_CVX, c * 256, (c + 1) * 256),
                                pk(P_CVX, c * 256, (c + 1) * 256))

            OT = fmv(P_OTX)

            def blocks_ga():
                for half in range(2):
                    def ev_ga(b, f):
                        S.op("act", lambda e: e.activation(out=GA[:, f, :], in_=PM[b][:, 0:TT], func=AF.Sigmoid,
                                                           bias=cv(V_BGA + f), scale=1.0),
                             [("pm", b), ("cvec",)], pk(P_GA, f * 256, (f + 1) * 256))
                    fm_block("in", C_GA + 512 * half, R_XTR, ev_ga)

            def blocks_r():
                for half in range(2):
                    def ev_r(b, f):
                        S.op("act", lambda e: e.activation(out=SR[:, f, :], in_=PM[b][:, 0:TT], func=AF.Silu,
                                                           bias=cv(V_BR + f), scale=1.0),
                             [("pm", b), ("cvec",)], pk(P_SRX, f * 256, (f + 1) * 256))
                    fm_block("in", C_R + 512 * half, R_XTR, ev_r)

            def blocks_gb():
                for half in range(2):
                    def ev_gb(b, f):
                        S.op("act", lambda e: e.activation(out=GB[:, f, :], in_=PM[b][:, 0:TT], func=AF.Sigmoid,
                                                           bias=cv(V_BGB + f), scale=1.0),
                             [("pm", b), ("cvec",)], pk(P_GBX, f * 256, (f + 1) * 256))
                    fm_block("in", C_GB + 512 * half, R_XTR, ev_gb)

            def gla_kv(n):
                s, hf = n // 2, n % 2
                p0 = hf * 64
                g = it * NCHK + n
                So, Sn = ST[g % 2], ST[(g + 1) % 2]
                for h in range(4):
                    S.op("pe", lambda e, s=s, h=h, p0=p0: e.matmul(
                        PK[:, h * 256:(h + 1) * 256], KT[p0:p0 + 64, s, h * 128:(h + 1) * 128],
                        VT[p0:p0 + 64, s, h * 256:(h + 1) * 256], start=True, stop=True),
                        rk(R_KT, 1024 + s * 512, 1024 + (s + 1) * 512) + rk(R_V, s * 1024, (s + 1) * 1024),
                        [("pk", h // 2)])
                for h in range(4):
                    dcol = (s * 4 + h) * 2 + hf
                    S.op("dve", lambda e, h=h, dcol=dcol, So=So, Sn=Sn: e.scalar_tensor_tensor(
                        out=Sn[:, h, :], in0=So[:, h, :], scalar=DEC[:, dcol:dcol + 1],
                        in1=PK[:, h * 256:(h + 1) * 256], op0=ALU.mult, op1=ALU.add),
                        [("pk", h // 2), ("dec",), ("st", g % 2, h)],
                        [("st", (g + 1) % 2, h)])

            def gla_o(n):
                g = it * NCHK + n
                Sn = ST[(g + 1) % 2]
                for h in range(4):
                    for j in range(2):
                        hj = h * 2 + j
                        S.op("pe", lambda e, h=h, j=j, hj=hj, n=n, Sn=Sn: e.matmul(
                            PO[:, hj * 64:(hj + 1) * 64], Sn[:, h, j * 128:(j + 1) * 128],
                            QT[:, h, n * 64:(n + 1) * 64], start=True, stop=True),
                            [("st", (g + 1) % 2, h)] + pk(P_QT, h * 256, (h + 1) * 256), [("po",)])
                S.op("act", lambda e, n=n: e.activation(
                    out=OT[:, :, n * 64:(n + 1) * 64], in_=PO[:, :].rearrange("p (c t) -> p c t", c=8),
                    func=AF.Copy, scale=float(128.0 ** -0.5)),
                    [("po",)], pk(P_OTX))

            SQ2 = rmv(R_SQ)
            RS = slot(P_QT, 1024, 2048).rearrange("p (h t) -> p h t", h=4)
            K_RS = pk(P_QT, 1024, 2048)
            OGR = rmv(R_OGR)
            MR = rmv(R_MR)

            def h_pre():
                S.op("act", lambda e: e.activation(out=rslot(R_SQ), in_=slot(P_OTX), func=AF.Square),
                     pk(P_OTX), rk(R_SQ))
                for hp in range(2):
                    for hh in range(2):
                        h = hp * 2 + hh
                        for j in range(2):
                            S.op("pe", lambda e, h=h, hh=hh, j=j: e.matmul(
                                PS_[:, hh * TT:(hh + 1) * TT], ONESR[:, :], SQ2[:, h * 2 + j, :],
                                start=(j == 0), stop=(j == 1)),
                                [("onesr",)] + rk(R_SQ), [("pstat",)])
                    S.op("act", lambda e, hp=hp: e.activation(
                        out=RS[:, hp * 2:hp * 2 + 2, :], in_=PS_[:, :].rearrange("p (h t) -> p h t", h=2),
                        func=AF.Sqrt, bias=EPS[:, 1:2], scale=1.0 / 256.0),
                        [("pstat",), ("eps",)], pk(P_QT, 1024 + hp * 512, 1024 + (hp + 1) * 512))

            def h_dve():
                S.op("dve", lambda e: e.reciprocal(out=RS, in_=RS), K_RS, K_RS)
                for h in range(4):
                    S.op("dve", lambda e, h=h: e.tensor_tensor(
                        out=OT[:, 2 * h:2 * h + 2, :], in0=OT[:, 2 * h:2 * h + 2, :],
                        in1=RS[:, h, :].unsqueeze(1).to_broadcast([128, 2, TT]), op=ALU.mult),
                        pk(P_OTX, h * 512, (h + 1) * 512) + K_RS, pk(P_OTX, h * 512, (h + 1) * 512))
                for hj in range(8):
                    S.op("dve", lambda e, hj=hj: e.scalar_tensor_tensor(
                        out=OGR[:, hj, :], in0=OT[:, hj, :], scalar=cv(V_NG + hj), in1=SR[:, hj, :],
                        op0=ALU.mult, op1=ALU.mult),
                        pk(P_OTX, hj * 256, (hj + 1) * 256) + pk(P_SRX, hj * 256, (hj + 1) * 256) + [("cvec",)],
                        rk(R_OGR, hj * 256, (hj + 1) * 256))

            def blocks_gla():
                for half in range(2):
                    def ev_t(b, f):
                        S.op("dve", lambda e: e.tensor_tensor(out=GB[:, f, :], in0=PM[b][:, 0:TT],
                                                              in1=GB[:, f, :], op=ALU.mult),
                             [("pm", b)] + pk(P_GBX, f * 256, (f + 1) * 256), pk(P_GBX, f * 256, (f + 1) * 256))
                    fm_block("gla", 512 * half, R_OGR, ev_t)

            conv_part(0, 4)
            gla_kv(0)
            blocks_ga()
            gla_o(0)
            gla_kv(1)
            conv_part(4, 12)
            blocks_r()
            gla_o(1)
            gla_kv(2)
            conv_part(12, 20)
            blocks_gb()
            gla_o(2)
            gla_kv(3)
            conv_part(20, 24)
            gla_o(3)
            stage("ot", it, [("ot", OT, pk(P_OTX)), ("st0", ST[0][:, :, :], [("st", 0, h) for h in range(4)]),
                             ("st1", ST[1][:, :, :], [("st", 1, h) for h in range(4)])])
            h_pre()
            conv_part(24, 28)
            h_dve()
            if it + 1 < NTILE:
                load_x(it + 1)
            stage("ogr", it, [("ogr", OGR, rk(R_OGR)), ("sr", SR, pk(P_SRX))])
            blocks_gla()
            conv_part(28, 31)
            stage("conv", it, [("cv", CVT, pk(P_CVX))])
            ln_stats(P_CVX, 0)
            CSR = rmv(R_CSR)
            for c in range(8):
                S.op("act", lambda e, c=c: e.activation(out=CSR[:, c, :], in_=CVT[:, c, :], func=AF.Silu,
                                                        bias=cv(V_CLB + c), scale=cv(V_CLG + c)),
                     pk(P_CVX, c * 256, (c + 1) * 256) + [("cvec",)], rk(R_CSR, c * 256, (c + 1) * 256))

            stage("csr", it, [("csr", CSR, rk(R_CSR))])
            for half in range(2):
                def ev_m(b, f):
                    S.op("dve", lambda e: e.scalar_tensor_tensor(
                        out=GA[:, f, :], in0=PM[b][:, 0:TT], scalar=cv(V_BCO + f), in1=GA[:, f, :],
                        op0=ALU.add, op1=ALU.mult),
                        [("pm", b), ("cvec",)] + pk(P_GA, f * 256, (f + 1) * 256), pk(P_GA, f * 256, (f + 1) * 256))
                    S.op("dve", lambda e: e.tensor_tensor(out=MR[:, f, :], in0=GA[:, f, :], in1=GB[:, f, :],
                                                          op=ALU.add),
                         pk(P_GA, f * 256, (f + 1) * 256) + pk(P_GBX, f * 256, (f + 1) * 256),
                         rk(R_MR, f * 256, (f + 1) * 256))
                fm_block("co", 512 * half, R_CSR, ev_m)

            stage("mr", it, [("mr", MR, rk(R_MR))])
            Z = fmv(P_ZX)
            for half in range(2):
                def ev_z(b, f):
                    S.op("act", lambda e: e.activation(out=Z[:, f, :], in_=PM[b][:, 0:TT], func=AF.Identity,
                                                       bias=cv(V_BO + f), scale=1.0),
                         [("pm", b), ("cvec",)], pk(P_ZX, f * 256, (f + 1) * 256))
                    S.op("dve", lambda e: e.scalar_tensor_tensor(
                        out=Z[:, f, :], in0=XT32[:, f, :], scalar=ALPHA, in1=Z[:, f, :],
                        op0=ALU.mult, op1=ALU.add),
                        pk(P_XT32, f * 256, (f + 1) * 256) + pk(P_ZX, f * 256, (f + 1) * 256),
                        pk(P_ZX, f * 256, (f + 1) * 256))
                fm_block("out", 512 * half, R_MR, ev_z)
            ln_stats(P_ZX, 0)
            X1T = fmv(P_X1TX)
            for c in range(8):
                S.op("act", lambda e, c=c: e.activation(out=X1T[:, c, :], in_=Z[:, c, :], func=AF.Identity,
                                                        bias=cv(V_L1B + c), scale=cv(V_L1G + c)),
                     pk(P_ZX, c * 256, (c + 1) * 256) + [("cvec",)], pk(P_X1TX, c * 256, (c + 1) * 256))

            stage("x1t", it, [("x1t", X1T, pk(P_X1TX))])
            yield "tailA"
            for s in range(NSUB):
                for k in range(8):
                    S.op("pe", lambda e, s=s, k=k: e.matmul(PS_[:, 0:72], X1T[:, k, s * 128:(s + 1) * 128],
                                                            WRT[:, k, :], start=(k == 0), stop=(k == 7)),
                         pk(P_X1TX) + [("wrt",)], [("pstat",)])
                S.op("dve", lambda e, s=s: e.tensor_tensor(out=LG[:, s, :], in0=PS_[:, 0:72], in1=BREP[:, :],
                                                           op=ALU.add),
                     [("pstat",), ("brep",)], [("lg", s)])
            X1K = slot(P_X1KX).rearrange("p (s d) -> p s d", s=NSUB)
            for s in range(NSUB):
                for cg in range(2):
                    for cc in range(4):
                        c = cg * 4 + cc
                        S.op("pe", lambda e, s=s, c=c, cc=cc: e.transpose(
                            PO[:, cc * 128:(cc + 1) * 128], X1T[:, c, s * 128:(s + 1) * 128], IDENT),
                            pk(P_X1TX) + [("const",)], [("po",)])
                    S.op("act", lambda e, s=s, cg=cg: e.activation(
                        out=X1K[:, s, cg * 512:(cg + 1) * 512], in_=PO[:, :], func=AF.Copy),
                        [("po",)], pk(P_X1KX, s * 1024 + cg * 512, s * 1024 + (cg + 1) * 512))
            S.dma("sp", lambda e, t0=t0: e.dma_start(
                out=x1d[t0:t0 + TT, :].rearrange("(s p) d -> p s d", p=128), in_=X1K),
                pk(P_X1KX), [("x1d", it)], "x1st")

            yield "tailB1"
            for s in range(NSUB):
                js = it * NSUB + s
                rt = R_
                lg = LG[:, s, :]
                gl = LG[:, s, 0:8]
                el3 = LG[:, s, 8:72].rearrange("p (g e) -> p g e", g=8)

                def D_(fn, reads, writes):
                    S.op("dve", fn, [("r", n_) if isinstance(n_, str) else n_ for n_ in reads],
                         [("r", n_) if isinstance(n_, str) else n_ for n_ in writes])

                def A_(fn, reads, writes):
                    S.op("act", fn, [("r", n_) if isinstance(n_, str) else n_ for n_ in reads],
                         [("r", n_) if isinstance(n_, str) else n_ for n_ in writes])
                LGK = ("lg", s)
                D_(lambda e: e.reduce_max(out=rt["gmax"][:, :], in_=gl, axis=AX.X), [LGK], ["gmax"])
                D_(lambda e: e.tensor_scalar(out=rt["ohg"][:, :], in0=gl, scalar1=rt["gmax"][:, 0:1], scalar2=None,
                                             op0=ALU.is_equal), [LGK, "gmax"], ["ohg"])
                D_(lambda e: e.tensor_scalar(out=rt["ngmax"][:, :], in0=rt["gmax"][:, :], scalar1=-1.0,
                                             scalar2=None, op0=ALU.mult), ["gmax"], ["ngmax"])
                A_(lambda e: e.activation(out=rt["ge"][:, :], in_=gl, func=AF.Exp, bias=rt["ngmax"][:, 0:1],
                                          scale=1.0), [LGK, "ngmax"], ["ge"])
                D_(lambda e: e.reduce_sum(out=rt["gsum"][:, :], in_=rt["ge"][:, :], axis=AX.X), ["ge"], ["gsum"])
                D_(lambda e: e.reciprocal(out=rt["gw"][:, :], in_=rt["gsum"][:, :]), ["gsum"], ["gw"])
                t64_3 = rt["t64"][:, :].rearrange("p (g e) -> p g e", g=8)
                D_(lambda e: e.tensor_tensor(out=t64_3, in0=el3,
                                             in1=rt["ohg"][:, :].unsqueeze(2).to_broadcast([128, 8, 8]),
                                             op=ALU.mult), [LGK, "ohg"], ["t64"])
                D_(lambda e: e.tensor_reduce(out=rt["esel"][:, :],
                                             in_=rt["t64"][:, :].rearrange("p (g e) -> p e g", g=8),
                                             axis=AX.X, op=ALU.add), ["t64"], ["esel"])
                D_(lambda e: e.reduce_max(out=rt["m1"][:, :], in_=rt["esel"][:, :], axis=AX.X), ["esel"], ["m1"])
                D_(lambda e: e.tensor_scalar(out=rt["oh1"][:, :], in0=rt["esel"][:, :], scalar1=rt["m1"][:, 0:1],
                                             scalar2=None, op0=ALU.is_equal), ["esel", "m1"], ["oh1"])
                D_(lambda e: e.scalar_tensor_tensor(out=rt["es2"][:, :], in0=rt["oh1"][:, :], scalar=-1e30,
                                                    in1=rt["esel"][:, :], op0=ALU.mult, op1=ALU.add),
                   ["oh1", "esel"], ["es2"])
                D_(lambda e: e.reduce_max(out=rt["m2"][:, :], in_=rt["es2"][:, :], axis=AX.X), ["es2"], ["m2"])
                D_(lambda e: e.tensor_scalar(out=rt["oh2"][:, :], in0=rt["es2"][:, :], scalar1=rt["m2"][:, 0:1],
                                             scalar2=None, op0=ALU.is_equal), ["es2", "m2"], ["oh2"])
                D_(lambda e: e.tensor_tensor(out=rt["d"][:, :], in0=rt["m2"][:, :], in1=rt["m1"][:, :],
                                             op=ALU.subtract), ["m1", "m2"], ["d"])
                A_(lambda e: e.activation(out=rt["e2"][:, :], in_=rt["d"][:, :], func=AF.Exp), ["d"], ["e2"])
                D_(lambda e: e.tensor_scalar(out=rt["den"][:, :], in0=rt["e2"][:, :], scalar1=1.0, scalar2=None,
                                             op0=ALU.add), ["e2"], ["den"])
                D_(lambda e: e.reciprocal(out=rt["p1"][:, :], in_=rt["den"][:, :]), ["den"], ["p1"])
                D_(lambda e: e.tensor_tensor(out=rt["p2"][:, :], in0=rt["e2"][:, :], in1=rt["p1"][:, :],
                                             op=ALU.mult), ["e2", "p1"], ["p2"])
                for nm, oh in (("oha", "oh1"), ("ohb", "oh2")):
                    D_(lambda e, nm=nm, oh=oh: e.tensor_tensor(
                        out=rt[nm][:, :].rearrange("p (g e) -> p g e", g=8),
                        in0=rt["ohg"][:, :].unsqueeze(2).to_broadcast([128, 8, 8]),
                        in1=rt[oh][:, :].unsqueeze(1).to_broadcast([128, 8, 8]), op=ALU.mult),
                        ["ohg", oh], [nm])
                D_(lambda e: e.tensor_tensor(out=rt["ohs"][:, :], in0=rt["oha"][:, :], in1=rt["ohb"][:, :],
                                             op=ALU.add), ["oha", "ohb"], ["ohs"])
                S.op("pe", lambda e: e.matmul(PS_[:, 128:192], TRIS, rt["ohs"][:, :], start=True, stop=False),
                     [("const",), ("r", "ohs")], [("pstat",)])
                S.op("pe", lambda e: e.matmul(PS_[:, 128:192], ONES[:, :], RUN[:, :], start=False, stop=True),
                     [("ones",), ("run",)], [("pstat",)])
                D_(lambda e: e.tensor_copy(out=rt["cnt"][:, :], in_=PS_[:, 128:192]), [("pstat",)], ["cnt"])
                D_(lambda e: e.tensor_tensor(out=RUN[:, :], in0=RUN[:, :], in1=rt["ohs"][:, :], op=ALU.add),
                   [("run",), "ohs"], [("run",)])
                for a, nm in enumerate(("oha", "ohb")):
                    D_(lambda e, nm=nm: e.tensor_tensor(out=rt["t64"][:, :], in0=rt["cnt"][:, :],
                                                        in1=rt[nm][:, :], op=ALU.mult), ["cnt", nm], ["t64"])
                    D_(lambda e, a=a: e.reduce_sum(out=rt["pos"][:, a:a + 1], in_=rt["t64"][:, :], axis=AX.X),
                       ["t64", "pos"], ["pos"])
                    D_(lambda e, nm=nm: e.tensor_tensor(out=rt["t64"][:, :], in0=IOTA, in1=rt[nm][:, :],
                                                        op=ALU.mult), [("const",), nm], ["t64"])
                    D_(lambda e, a=a: e.reduce_sum(out=rt["eid"][:, a:a + 1], in_=rt["t64"][:, :], axis=AX.X),
                       ["t64", "eid"], ["eid"])
                D_(lambda e: e.tensor_scalar(out=rt["val"][:, :], in0=rt["pos"][:, :], scalar1=float(CAP),
                                             scalar2=None, op0=ALU.is_lt), ["pos"], ["val"])
                D_(lambda e: e.scalar_tensor_tensor(out=rt["slot"][:, :], in0=rt["eid"][:, :], scalar=float(CAP),
                                                    in1=rt["pos"][:, :], op0=ALU.mult, op1=ALU.add),
                   ["eid", "pos"], ["slot"])
                D_(lambda e: e.scalar_tensor_tensor(out=rt["sidf"][:, :], in0=rt["slot"][:, :], scalar=-1.0e6,
                                                    in1=rt["val"][:, :], op0=ALU.add, op1=ALU.mult),
                   ["slot", "val"], ["sidf"])
                D_(lambda e: e.tensor_scalar(out=rt["sidf"][:, :], in0=rt["sidf"][:, :], scalar1=1.0e6,
                                             scalar2=None, op0=ALU.add), ["sidf"], ["sidf"])
                D_(lambda e: e.tensor_tensor(out=rt["tmp2"][:, :], in0=rt["pos"][:, :], in1=rt["val"][:, :],
                                             op=ALU.mult), ["pos", "val"], ["tmp2"])
                D_(lambda e: e.scalar_tensor_tensor(out=rt["gidf"][:, :], in0=rt["eid"][:, :], scalar=float(CAP),
                                                    in1=rt["tmp2"][:, :], op0=ALU.mult, op1=ALU.add),
                   ["eid", "tmp2"], ["gidf"])
                D_(lambda e, js=js: e.tensor_copy(out=SIDX[:, js, :], in_=rt["sidf"][:, :]), ["sidf"],
                   [("sidx", js)])
                D_(lambda e, js=js: e.tensor_copy(out=GIDX[:, js, :], in_=rt["gidf"][:, :]), ["gidf"],
                   [("gidx", js)])
                for a, pn in enumerate(("p1", "p2")):
                    D_(lambda e, a=a, pn=pn, js=js: e.scalar_tensor_tensor(
                        out=WAB[:, js, a:a + 1], in0=rt[pn][:, :], scalar=rt["gw"][:, 0:1],
                        in1=rt["val"][:, a:a + 1], op0=ALU.mult, op1=ALU.mult),
                        [pn, "gw", "val", ("wab", js)], [("wab", js)])
                if DEBUG:
                    D_(lambda e, js=js: e.tensor_copy(out=RTF[:, js, 0:2], in_=rt["gidf"][:, :]),
                       ["gidf", ("rtf",)], [("rtf",)])
                    D_(lambda e, js=js: e.tensor_copy(out=RTF[:, js, 2:4], in_=WAB[:, js, :]),
                       [("wab", js), ("rtf",)], [("rtf",)])
                for a in range(2):
                    S.dma("pool", lambda e, js=js, a=a, s=s: e.indirect_dma_start(
                        out=xs_d, out_offset=bass.IndirectOffsetOnAxis(ap=SIDX[:, js, a:a + 1], axis=0),
                        in_=X1K[:, s, :], in_offset=None, bounds_check=NE * CAP - 1, oob_is_err=False),
                        [("sidx", js)] + pk(P_X1KX, s * 1024, (s + 1) * 1024), [("xs",)], f"sc{a}")

        def phase23():
            def ew(setid, which):
                if setid == 0:
                    return WBT[which][:, :], [("WB", which)]
                return RPOOL[:, which * 2 * SL:(which + 1) * 2 * SL], rk(2 * which) + rk(2 * which + 1)

            XSEs = [slot(0, 0, 1024), slot(0, 1024, 2048)]
            K_XSEs = [pk(0, 0, 1024), pk(0, 1024, 2048)]
            YEs = [slot(1, 0, 1024), slot(1, 1024, 2048)]
            K_YEs = [pk(1, 0, 1024), pk(1, 1024, 2048)]
            HDs = [slot(2, 0, 512), slot(2, 512, 1024)]
            K_HDs = [pk(2, 0, 512), pk(2, 512, 1024)]
            XST = rslot(R_X, 0, 1024).rearrange("p (k t) -> p k t", k=8)
            HDT = rslot(R_X, 1024, 1536).rearrange("p (k t) -> p k t", k=4)
            K_XST, K_HDT = rk(R_X, 0, 1024), rk(R_X, 1024, 1536)

            def load_expert(E):
                st_ = E % 2
                for which, (src, shp) in enumerate(((w1, 8), (w3, 8), (w2, 4))):
                    buf, keys = ew(st_, which)
                    dst = buf.rearrange("p (k n) -> p k n", k=shp)
                    S.dma("pool", lambda e, dst=dst, src=src, E=E: e.dma_start(
                        out=dst, in_=src[E].rearrange("(k p) n -> p k n", p=128)), [], keys, f"ew{st_}{which}")

            def load_xse(E):
                par = E % 2
                S.dma("sp", lambda e, E=E, par=par: e.dma_start(out=XSEs[par], in_=xs_d[E * CAP:(E + 1) * CAP, :]),
                      [("xs",)], K_XSEs[par], f"xse{par}")

            def stage_A(E):
                par = E % 2
                XSE = XSEs[par]
                for cg in range(2):
                    for cc in range(4):
                        c = cg * 4 + cc
                        S.op("pe", lambda e, c=c, cc=cc, XSE=XSE: e.transpose(
                            PO[:, cc * 128:(cc + 1) * 128], XSE[:, c * 128:(c + 1) * 128], IDENT),
                            K_XSEs[par] + [("const",)], [("po",)])
                    S.op("act", lambda e, cg=cg: e.activation(
                        out=XST[:, cg * 4:cg * 4 + 4, :], in_=PO[:, :].rearrange("p (c t) -> p c t", c=4),
                        func=AF.Copy), [("po",)], K_XST)
                if E + 2 < NE:
                    load_xse(E + 2)

            def stage_B(E):
                st_ = E % 2
                W1, k1 = ew(st_, 0)
                W3, k3 = ew(st_, 1)
                W1 = W1.rearrange("p (k n) -> p k n", k=8)
                W3 = W3.rearrange("p (k n) -> p k n", k=8)
                HD = HDs[st_]
                b1 = next_pm()
                b3 = next_pm()
                for (b, W, kk) in ((b1, W1, k1), (b3, W3, k3)):
                    for k in range(8):
                        S.op("pe", lambda e, b=b, W=W, k=k: e.matmul(PM[b][:, :], XST[:, k, :], W[:, k, :],
                                                                     start=(k == 0), stop=(k == 7)),
                             K_XST + kk, [("pm", b)])
                S.op("act", lambda e, b1=b1, HD=HD: e.activation(out=HD, in_=PM[b1][:, :], func=AF.Silu),
                     [("pm", b1)], K_HDs[st_])
                S.op("dve", lambda e, b3=b3, HD=HD: e.tensor_tensor(out=HD, in0=HD, in1=PM[b3][:, :], op=ALU.mult),
                     [("pm", b3)] + K_HDs[st_], K_HDs[st_])

            def stage_C(E):
                st_ = E % 2
                HD = HDs[st_]
                for j in range(4):
                    S.op("pe", lambda e, j=j, HD=HD: e.transpose(PO[:, j * 128:(j + 1) * 128],
                                                                 HD[:, j * 128:(j + 1) * 128], IDENT),
                         K_HDs[st_] + [("const",)], [("po",)])
                S.op("act", lambda e: e.activation(out=HDT, in_=PO[:, :].rearrange("p (c t) -> p c t", c=4),
                                                   func=AF.Copy), [("po",)], K_HDT)

            def stage_D(E):
                st_ = E % 2
                W2, k2 = ew(st_, 2)
                W2 = W2.rearrange("p (k n) -> p k n", k=4)
                YE = YEs[st_]
                for half in range(2):
                    b = next_pm()
                    for j in range(4):
                        S.op("pe", lambda e, b=b, j=j, half=half, W2=W2: e.matmul(
                            PM[b][:, :], HDT[:, j, :], W2[:, j, half * 512:(half + 1) * 512],
                            start=(j == 0), stop=(j == 3)), K_HDT + k2, [("pm", b)])
                    if half == 0:
                        S.op("act", lambda e, b=b, YE=YE: e.activation(out=YE[:, 0:512], in_=PM[b][:, :],
                                                                       func=AF.Copy),
                             [("pm", b)], pk(1, st_ * 1024, st_ * 1024 + 512))
                    else:
                        S.op("dve", lambda e, b=b, YE=YE: e.tensor_copy(out=YE[:, 512:1024], in_=PM[b][:, :]),
                             [("pm", b)], pk(1, st_ * 1024 + 512, st_ * 1024 + 1024))
                S.dma("sp", lambda e, E=E, YE=YE: e.dma_start(out=ys_d[E * CAP:(E + 1) * CAP, :], in_=YE),
                      K_YEs[st_], [("ys",)], f"yst{st_}")
                if E + 2 < NE:
                    load_expert(E + 2)

            load_expert(0)
            load_expert(1)
            load_xse(0)
            load_xse(1)
            stage_A(0)
            stage_B(0)
            stage_A(1)
            for E in range(NE):
                stage_C(E)
                if E + 1 < NE:
                    stage_B(E + 1)
                stage_D(E)
                if E + 2 < NE:
                    stage_A(E + 2)

            G2B2 = slot(2)
            S.dma("sp", lambda e: e.dma_start(out=G2B2, in_=g2b2_d), [], pk(2), "c0")
            if DEBUG:
                S.dma("sp", lambda e: e.dma_start(out=rtd, in_=RTF[:, :, :].rearrange("p a b -> p (a b)")),
                      [("rtf",)], [("rtd",)], "c1")
            out_evs = []
            NPAR = 4
            PBASE = (3 * SL, 3 * SL + 3072, 3 * SL + 6144, 0)

            def pkabs(lo, hi):
                return [("P", w // SL, (w % SL) // 256) for w in range(lo - lo % 256, hi, 256)]

            def bufs(js):
                b0 = PBASE[js % NPAR]
                YA, YB, X1 = POOL[:, b0:b0 + 1024], POOL[:, b0 + 1024:b0 + 2048], POOL[:, b0 + 2048:b0 + 3072]
                return YA, YB, X1, pkabs(b0, b0 + 1024), pkabs(b0 + 1024, b0 + 2048), pkabs(b0 + 2048, b0 + 3072)

            def stv(js):
                return {n: L2S[:, js, i_:i_ + 1] for i_, n in enumerate(("s1", "nmu", "vs", "sd", "rstd"))}

            def p3_load(js):
                par = js % NPAR
                YA, YB, X1, kYA, kYB, kX1 = bufs(js)
                S.dma("pool", lambda e: e.indirect_dma_start(
                    out=YA, out_offset=None, in_=ys_d,
                    in_offset=bass.IndirectOffsetOnAxis(ap=GIDX[:, js, 0:1], axis=0)),
                    [("ys",), ("gidx", js)], kYA, f"ga{par}")
                S.dma("pool", lambda e: e.indirect_dma_start(
                    out=YB, out_offset=None, in_=ys_d,
                    in_offset=bass.IndirectOffsetOnAxis(ap=GIDX[:, js, 1:2], axis=0)),
                    [("ys",), ("gidx", js)], kYB, f"gb{par}")
                S.dma("sp", lambda e: e.dma_start(out=X1, in_=x1d[js * 128:(js + 1) * 128, :]),
                      [("x1d", js // NSUB)], kX1, f"x1l{par}")

            def p3_s1(js):
                YA, YB, X1, kYA, kYB, kX1 = bufs(js)
                st = stv(js)
                S.op("act", lambda e: e.activation(out=YA, in_=YA, func=AF.Copy, scale=WAB[:, js, 0:1]),
                     kYA + [("wab", js)], kYA)
                S.op("dve", lambda e: e.scalar_tensor_tensor(
                    out=YB, in0=YB, scalar=WAB[:, js, 1:2], in1=YA, op0=ALU.mult, op1=ALU.add),
                    kYB + kYA + [("wab", js)], kYB)
                S.op("dve", lambda e: e.scalar_tensor_tensor(
                    out=YB, in0=X1, scalar=ALPHA, in1=YB, op0=ALU.mult, op1=ALU.add), kX1 + kYB, kYB)
                S.op("dve", lambda e: e.reduce_sum(out=st["s1"], in_=YB, axis=AX.X), kYB, [("l2", js, "s1")])
                S.op("dve", lambda e: e.tensor_scalar(out=st["nmu"], in0=st["s1"], scalar1=-1.0 / D,
                                                      scalar2=None, op0=ALU.mult),
                     [("l2", js, "s1")], [("l2", js, "nmu")])
                S.op("act", lambda e: e.activation(out=YA, in_=YB, func=AF.Square, bias=st["nmu"], scale=1.0),
                     kYB + kYA + [("l2", js, "nmu")], kYA)

            def p3_s2(js):
                YA, YB, X1, kYA, kYB, kX1 = bufs(js)
                st = stv(js)
                S.op("dve", lambda e: e.reduce_sum(out=st["vs"], in_=YA, axis=AX.X), kYA, [("l2", js, "vs")])
                S.op("act", lambda e: e.activation(out=st["sd"], in_=st["vs"], func=AF.Sqrt,
                                                   bias=EPS[:, 0:1], scale=1.0 / D),
                     [("l2", js, "vs"), ("eps",)], [("l2", js, "sd")])

            def p3_s3(js):
                par = js % NPAR
                YA, YB, X1, kYA, kYB, kX1 = bufs(js)
                st = stv(js)
                S.op("dve", lambda e: e.reciprocal(out=st["rstd"], in_=st["sd"]),
                     [("l2", js, "sd")], [("l2", js, "rstd")])
                S.op("dve", lambda e: e.scalar_tensor_tensor(
                    out=YB, in0=YB, scalar=st["nmu"], in1=G2B2[:, 0:D], op0=ALU.add, op1=ALU.mult),
                    kYB + [("l2", js, "nmu")] + pk(2), kYB)
                S.op("dve", lambda e: e.scalar_tensor_tensor(
                    out=YB, in0=YB, scalar=st["rstd"], in1=G2B2[:, D:2 * D], op0=ALU.mult, op1=ALU.add),
                    kYB + [("l2", js, "rstd")] + pk(2), kYB)
                ev = S.dma("sp", lambda e: e.dma_start(out=out[js * 128:(js + 1) * 128, :], in_=YB),
                           kYB, [("out", js)], f"ost{par}")
                out_evs.append(ev)

            for js in range(NPAR):
                p3_load(js)
            for step in range(16 + 2):
                if step < 16:
                    p3_s1(step)
                if 0 <= step - 1 < 16:
                    p3_s2(step - 1)
                if 0 <= step - 2 < 16:
                    p3_s3(step - 2)
                    if step - 2 + NPAR < 16:
                        p3_load(step - 2 + NPAR)
            for ev in out_evs:
                S.wait_event("sp", ev)
            if DEBUG:
                for k_ in ("x1st", "c1"):
                    ent = S.dsem[k_]
                    S.wait_event("sp", ("d_" + k_, ent[0], 16 * ent[1], None))


        try:
            gens = [phase1_tile(it) for it in range(NTILE)]
            next(gens[0])
            next(gens[0])
            for it in range(NTILE):
                next(gens[it])
                next(gens[it])
                if it + 1 < NTILE:
                    next(gens[it + 1])
                for _ in gens[it]:
                    pass
                if it + 1 < NTILE:
                    next(gens[it + 1])
            phase23()
        except _Stop:
            for ev in dbg_evs:
                S.wait_event("pool", ev)

        block = es.enter_context(nc.Block())
        S.emit_all(block)
    return nc


def host_prep(inp):
    f = lambda a: np.ascontiguousarray(np.asarray(a, dtype=np.float32))
    b_in = f(inp["b_in"])[0]

    def colvec(v):
        return v.reshape(-1, 128).T

    cvec = np.zeros((128, NVEC), np.float32)
    cvec[:, V_BA:V_BA + 8] = colvec(b_in[C_A:C_A + 1024])
    cvec[:, V_BG:V_BG + 8] = colvec(b_in[C_G:C_G + 1024])
    cvec[:, V_BQ:V_BQ + 4] = colvec(b_in[C_Q:C_Q + 512])
    cvec[:, V_BR:V_BR + 8] = colvec(b_in[C_R:C_R + 1024])
    cvec[:, V_BGA:V_BGA + 8] = colvec(b_in[C_GA:C_GA + 1024])
    cvec[:, V_BGB:V_BGB + 8] = colvec(b_in[C_GB:C_GB + 1024])
    cvec[0:16, V_BF] = b_in[C_F:C_F + 16]
    cvec[:, V_CB:V_CB + 8] = colvec(f(inp["conv_b"])[0])
    cvec[:, V_CLG:V_CLG + 8] = colvec(f(inp["conv_ln_g"])[0])
    cvec[:, V_CLB:V_CLB + 8] = colvec(f(inp["conv_ln_b"])[0])
    cvec[:, V_BCO:V_BCO + 8] = colvec(f(inp["b_conv_out"])[0])
    cvec[:, V_NG:V_NG + 8] = colvec(f(inp["gla_norm_g"])[0].reshape(-1))
    cvec[:, V_BO:V_BO + 8] = colvec(f(inp["b_out"])[0])
    cvec[:, V_L1G:V_L1G + 8] = colvec(f(inp["ln1_g"])[0])
    cvec[:, V_L1B:V_L1B + 8] = colvec(f(inp["ln1_b"])[0])
    cw = f(inp["conv_w"])[0]
    cvec[:, V_CW:V_CW + 248] = cw.T.reshape(8, 128, 31).transpose(1, 0, 2).reshape(128, 248)

    consts = np.zeros((128, NCONST), np.float32)
    consts[:, K_ID:K_ID + 128] = np.eye(128, dtype=np.float32)
    sp = np.arange(128)[:, None]
    tt = np.arange(128)[None, :]
    consts[:, K_TRIU:K_TRIU + 128] = ((sp // 64 == tt // 64) & (sp > tt)).astype(np.float32)
    consts[:, K_TRIS:K_TRIS + 128] = (sp < tt).astype(np.float32)
    consts[:, K_IND:K_IND + 2] = (sp // 64 == np.arange(2)[None, :]).astype(np.float32)
    consts[:, K_IOTA:K_IOTA + 64] = np.arange(64, dtype=np.float32)[None, :]

    brow = np.zeros((4, 512), np.float32)
    brow[0] = b_in[C_K:C_K + 512]
    brow[1] = b_in[C_V:C_V + 512]
    brow[2] = b_in[C_V + 512:C_V + 1024]
    brow[3] = f(inp["b_gate_up"])[0]
    brep = np.tile(np.concatenate([f(inp["b_router_group"])[0], f(inp["b_router_expert"])[0]])[None, :], (128, 1))
    g2b2 = np.tile(np.concatenate([f(inp["ln2_g"])[0], f(inp["ln2_b"])[0]])[None, :], (128, 1))
    w_r = np.concatenate([f(inp["w_router_group"])[0], f(inp["w_router_expert"])[0]], axis=1)
    shared = {
        "w_in": f(inp["w_in"])[0], "w_co": f(inp["w_conv_out"])[0], "w_gla": f(inp["w_gla_out"])[0],
        "w_out": f(inp["w_out"])[0], "w_gu": f(inp["w_gate_up"])[0], "w_r": np.ascontiguousarray(w_r),
        "w1": f(inp["w1"])[0], "w3": f(inp["w3"])[0], "w2": f(inp["w2"])[0],
        "cvec": cvec, "consts": consts, "brow": brow, "brep": np.ascontiguousarray(brep),
        "g2b2": np.ascontiguousarray(g2b2),
    }
    xs = f(inp["x"])
    return [dict(shared, x=np.ascontiguousarray(xs[b])) for b in range(NCORES)]


_NC_CACHE = {}


def kernel(**inputs):
    in_maps = host_prep(inputs)
    if "nc" not in _NC_CACHE:
        _NC_CACHE["nc"] = build_nc()
    nc = _NC_CACHE["nc"]
    res = run_bass_kernel_spmd(nc, in_maps, core_ids=list(range(NCORES)))
    kernel.last_results = res
    return np.stack([np.asarray(r["out"], dtype=np.float32) for r in res.results], axis=0)
```
